# Optimizing a Trainium2 kernel written in Bass

```python
import jax
import jax.numpy as jnp
from jax import lax
import numpy as np

D_MODEL = 1024
BATCH = 8
SEQ = 4096
DEPTH = 1

GRID_W = 64
CTX_LEN = 256

CONV_CH = 512
CONV_GROUPS = 8
GLA_HEADS = 4
GLA_DK = 64
GLA_DV = 128
GLA_KEY = GLA_HEADS * GLA_DK
GLA_VAL = GLA_HEADS * GLA_DV
GLA_GATE_RANK = 16
GLA_TAU = 16.0
GLA_CHUNK = 16
MIX_WIDTH = CONV_CH + GLA_VAL

OFF_AB = 0
OFF_AC = OFF_AB + CONV_CH
OFF_AX = OFF_AC + CONV_CH
OFF_Q = OFF_AX + CONV_CH
OFF_K = OFF_Q + GLA_KEY
OFF_V = OFF_K + GLA_KEY
OFF_R = OFF_V + GLA_VAL
OFF_GF = OFF_R + GLA_VAL
OFF_GB = OFF_GF + GLA_GATE_RANK
D_PROJ = OFF_GB + GLA_GATE_RANK

N_GROUPS = 4
EXPERTS_PER_GROUP = 4
N_EXPERTS = N_GROUPS * EXPERTS_PER_GROUP
TOP_K_IN_GROUP = 2
D_EXPERT = 512

LN_EPS = 1e-5
RMS_EPS = 1e-6
DEEPNORM_ALPHA = (2.0 * DEPTH) ** 0.25
DEEPNORM_BETA = (8.0 * DEPTH) ** -0.25

kernel_name = "hymba_conv_gla_hmoe_diffusion_layer"


def layer_norm(x, g, b):
    xf = x.astype(jnp.float32)
    mu = jnp.mean(xf, axis=-1, keepdims=True)
    var = jnp.mean(jnp.square(xf - mu), axis=-1, keepdims=True)
    y = (xf - mu) * lax.rsqrt(var + LN_EPS)
    return (y * g.astype(jnp.float32) + b.astype(jnp.float32)).astype(x.dtype)


def rms_norm(x, g):
    xf = x.astype(jnp.float32)
    y = xf * lax.rsqrt(jnp.mean(jnp.square(xf), axis=-1, keepdims=True) + RMS_EPS)
    return y * g.astype(jnp.float32)


def ada_params(cond, w_ada, b_ada):
    return jnp.split(jax.nn.silu(cond) @ w_ada + b_ada, 6, axis=-1)


def modulate(x, shift, scale):
    return x * (1.0 + scale) + shift


def post_norm_residual(x, y, gate, g, b):
    return layer_norm(DEEPNORM_ALPHA * x + gate * y, g, b)


def dwconv3(u, w, b):
    n = u.shape[-2]
    pad = [(0, 0)] * (u.ndim - 2) + [(1, 1), (0, 0)]
    up = jnp.pad(u, pad)
    return up[..., 0:n, :] * w[0] + up[..., 1:n + 1, :] * w[1] + up[..., 2:n + 2, :] * w[2] + b


def to_heads(a, d):
    bsz, t, _ = a.shape
    return a.reshape(bsz, t, GLA_HEADS, d).transpose(0, 2, 1, 3).astype(jnp.float32)


def log_decay(low, w2, b2):
    return jax.nn.log_sigmoid((low @ w2 + b2).astype(jnp.float32)) / GLA_TAU


def gla_chunked(q, k, v, g, s0):
    bsz, h, t, dk = q.shape
    dv = v.shape[-1]
    n = t // GLA_CHUNK
    q, k, g = (a.reshape(bsz, h, n, GLA_CHUNK, dk) for a in (q, k, g))
    v = v.reshape(bsz, h, n, GLA_CHUNK, dv)
    gc = jnp.cumsum(g, axis=3)
    g_last = gc[:, :, :, -1:, :]
    q_dec = q * jnp.exp(gc)
    k_inv = k * jnp.exp(-gc)
    k_end = k * jnp.exp(g_last - gc)
    lower = jnp.tril(jnp.ones((GLA_CHUNK, GLA_CHUNK), dtype=bool))
    scores = jnp.where(lower, jnp.einsum("bhncd,bhnsd->bhncs", q_dec, k_inv), 0.0)
    o_intra = jnp.einsum("bhncs,bhnse->bhnce", scores, v)
    u = jnp.einsum("bhncd,bhnce->bhnde", k_end, v)
    decay = jnp.exp(g_last[:, :, :, 0, :])

    def step(s, inp):
        dec_n, u_n = inp
        return dec_n[..., None] * s + u_n, s

    s_final, s_start = lax.scan(step, s0, (jnp.moveaxis(decay, 2, 0), jnp.moveaxis(u, 2, 0)))
    s_start = jnp.moveaxis(s_start, 0, 2)
    o_inter = jnp.einsum("bhncd,bhnde->bhnce", q_dec, s_start)
    return (o_intra + o_inter).reshape(bsz, h, t, dv), s_final


def flip_t(a):
    return jnp.flip(a, axis=2)


def gla_bidirectional(q, k, v, g_fwd, g_bwd, s0_fwd, s0_bwd):
    o_f, s_f = gla_chunked(q, k, v, g_fwd, s0_fwd)
    o_b, s_b = gla_chunked(flip_t(q), flip_t(k), flip_t(v), flip_t(g_bwd), s0_bwd)
    return o_f + flip_t(o_b), s_f, s_b


def gla_final_state(k, v, g):
    gc = jnp.cumsum(g, axis=2)
    w = jnp.exp(gc[:, :, -1:, :] - gc)
    return jnp.einsum("bhtd,bhte->bhde", k * w, v)


def hybrid_mixer(h, w_in, conv_w, conv_b, gw_f, gb_f, gw_b, gb_b, gla_norm_g, w_out,
                 s0_fwd, s0_bwd, on_grid):
    bsz, t, _ = h.shape
    p = h @ w_in
    a_b, a_c, a_x, q, k, v, r, low_f, low_b = jnp.split(
        p, [OFF_AC, OFF_AX, OFF_Q, OFF_K, OFF_V, OFF_R, OFF_GF, OFF_GB], axis=-1)
    u = a_c * a_x
    if on_grid:
        rows = t // GRID_W
        u_conv = dwconv3(u.reshape(bsz, rows, GRID_W, CONV_CH), conv_w, conv_b).reshape(bsz, t, CONV_CH)
    else:
        u_conv = dwconv3(u, conv_w, conv_b)
    y_a = a_b * u_conv
    qh = to_heads(q, GLA_DK) * (GLA_DK ** -0.5)
    kh = to_heads(k, GLA_DK)
    vh = to_heads(v, GLA_DV)
    gf = to_heads(log_decay(low_f, gw_f, gb_f), GLA_DK)
    gb = to_heads(log_decay(low_b, gw_b, gb_b), GLA_DK)
    o, s_f, s_b = gla_bidirectional(qh, kh, vh, gf, gb, s0_fwd, s0_bwd)
    o = rms_norm(o, gla_norm_g).transpose(0, 2, 1, 3).reshape(bsz, t, GLA_VAL)
    y_b = o.astype(h.dtype) * jax.nn.silu(r)
    y = jnp.concatenate([y_a, y_b], axis=-1) @ w_out
    return y, s_f, s_b


def context_gla_states(hc, w_in, gw_f, gb_f, gw_b, gb_b):
    kv = hc @ w_in[:, OFF_K:OFF_R]
    low = hc @ w_in[:, OFF_GF:D_PROJ]
    kh = to_heads(kv[..., :GLA_KEY], GLA_DK)
    vh = to_heads(kv[..., GLA_KEY:], GLA_DV)
    gf = to_heads(log_decay(low[..., :GLA_GATE_RANK], gw_f, gb_f), GLA_DK)
    gb = to_heads(log_decay(low[..., GLA_GATE_RANK:], gw_b, gb_b), GLA_DK)
    return gla_final_state(kh, vh, gf), gla_final_state(flip_t(kh), flip_t(vh), flip_t(gb))


def hier_moe(h, wgr, bgr, wer, ber, w1, w3, w2):
    bsz, t, d = h.shape
    tok = h.reshape(bsz * t, d)
    group_prob = jax.nn.softmax((tok @ wgr + bgr).astype(jnp.float32), axis=-1)
    p_g, g_idx = lax.top_k(group_prob, 1)
    exp_logits = (tok @ wer + ber).astype(jnp.float32).reshape(-1, N_GROUPS, EXPERTS_PER_GROUP)
    sel = jnp.take_along_axis(exp_logits, g_idx[:, :, None], axis=1)[:, 0]
    top_w, top_i = lax.top_k(jax.nn.softmax(sel, axis=-1), TOP_K_IN_GROUP)
    top_w = top_w / jnp.sum(top_w, axis=-1, keepdims=True)
    within = jnp.sum(jax.nn.one_hot(top_i, EXPERTS_PER_GROUP, dtype=jnp.float32) * top_w[..., None], axis=1)
    combine = jax.nn.one_hot(g_idx[:, 0], N_GROUPS, dtype=jnp.float32)[:, :, None] * (
        p_g[:, :, None] * within[:, None, :])
    combine = combine.reshape(-1, N_EXPERTS).astype(h.dtype)
    out = jnp.zeros_like(tok)
    for e in range(N_EXPERTS):
        act = jax.nn.silu(tok @ w1[e]) * (tok @ w3[e])
        out = out + combine[:, e:e + 1] * (act @ w2[e])
    return out.reshape(bsz, t, d)


def setup_inputs(seed: int = 0) -> dict:
    key = jax.random.key(seed)
    ks = jax.random.split(key, 28)
    f32 = jnp.float32
    L, D = DEPTH, D_MODEL

    def nrm(k, shape, s):
        return jax.random.normal(k, shape, f32) * s

    return {
        "x": nrm(ks[0], (BATCH, SEQ, D), 1.0),
        "c": nrm(ks[1], (BATCH, D), 1.0),
        "ctx": nrm(ks[2], (BATCH, CTX_LEN, D), 1.0),
        "c_ctx": nrm(ks[3], (D,), 1.0),
        "ln_in_g": 1.0 + nrm(ks[4], (D,), 0.02),
        "ln_in_b": nrm(ks[5], (D,), 0.02),
        "w_ada": nrm(ks[6], (L, D, 6 * D), D ** -0.5),
        "b_ada": nrm(ks[7], (L, 6 * D), 0.02),
        "w_in": nrm(ks[8], (L, D, D_PROJ), D ** -0.5),
        "conv_w": nrm(ks[9], (L, 3, CONV_CH), 3 ** -0.5),
        "conv_b": nrm(ks[10], (L, CONV_CH), 0.02),
        "gate_w2_fwd": nrm(ks[11], (L, GLA_GATE_RANK, GLA_KEY), GLA_GATE_RANK ** -0.5),
        "gate_b_fwd": nrm(ks[12], (L, GLA_KEY), 0.02),
        "gate_w2_bwd": nrm(ks[13], (L, GLA_GATE_RANK, GLA_KEY), GLA_GATE_RANK ** -0.5),
        "gate_b_bwd": nrm(ks[14], (L, GLA_KEY), 0.02),
        "gla_norm_g": 1.0 + nrm(ks[15], (L, GLA_DV), 0.02),
        "w_out": nrm(ks[16], (L, MIX_WIDTH, D), MIX_WIDTH ** -0.5 * DEEPNORM_BETA),
        "ln1_g": 1.0 + nrm(ks[17], (L, D), 0.02),
        "ln1_b": nrm(ks[18], (L, D), 0.02),
        "router_group_w": nrm(ks[19], (L, D, N_GROUPS), D ** -0.5),
        "router_group_b": nrm(ks[20], (L, N_GROUPS), 0.01),
        "router_expert_w": nrm(ks[21], (L, D, N_EXPERTS), D ** -0.5),
        "router_expert_b": nrm(ks[22], (L, N_EXPERTS), 0.01),
        "expert_w1": nrm(ks[23], (L, N_EXPERTS, D, D_EXPERT), D ** -0.5),
        "expert_w3": nrm(ks[24], (L, N_EXPERTS, D, D_EXPERT), D ** -0.5),
        "expert_w2": nrm(ks[25], (L, N_EXPERTS, D_EXPERT, D), D_EXPERT ** -0.5 * DEEPNORM_BETA),
        "ln2_g": 1.0 + nrm(ks[26], (L, D), 0.02),
        "ln2_b": nrm(ks[27], (L, D), 0.02),
    }


def reference(x, c, ctx, c_ctx, ln_in_g, ln_in_b, w_ada, b_ada, w_in, conv_w, conv_b,
              gate_w2_fwd, gate_b_fwd, gate_w2_bwd, gate_b_bwd, gla_norm_g, w_out,
              ln1_g, ln1_b, router_group_w, router_group_b, router_expert_w, router_expert_b,
              expert_w1, expert_w3, expert_w2, ln2_g, ln2_b):
    bsz = x.shape[0]
    x = layer_norm(x, ln_in_g, ln_in_b)
    xc = layer_norm(ctx, ln_in_g, ln_in_b)
    for l in range(DEPTH):
        last = l == DEPTH - 1
        sh1, sc1, g1, sh2, sc2, g2 = (m[:, None, :] for m in ada_params(c, w_ada[l], b_ada[l]))
        csh1, csc1, cg1, csh2, csc2, cg2 = ada_params(c_ctx, w_ada[l], b_ada[l])
        mix_w = (w_in[l], conv_w[l], conv_b[l], gate_w2_fwd[l], gate_b_fwd[l],
                 gate_w2_bwd[l], gate_b_bwd[l], gla_norm_g[l], w_out[l])
        moe_w = (router_group_w[l], router_group_b[l], router_expert_w[l], router_expert_b[l],
                 expert_w1[l], expert_w3[l], expert_w2[l])
        hc = modulate(xc, csh1, csc1)
        if last:
            s_f, s_b = context_gla_states(hc, w_in[l], gate_w2_fwd[l], gate_b_fwd[l],
                                          gate_w2_bwd[l], gate_b_bwd[l])
        else:
            zeros = jnp.zeros((bsz, GLA_HEADS, GLA_DK, GLA_DV), jnp.float32)
            yc, s_f, s_b = hybrid_mixer(hc, *mix_w, zeros, zeros, False)
            xc = post_norm_residual(xc, yc, cg1, ln1_g[l], ln1_b[l])
            xc = post_norm_residual(xc, hier_moe(modulate(xc, csh2, csc2), *moe_w), cg2, ln2_g[l], ln2_b[l])
        h = modulate(x, sh1, sc1)
        y, _, _ = hybrid_mixer(h, *mix_w, s_f, s_b, True)
        x = post_norm_residual(x, y, g1, ln1_g[l], ln1_b[l])
        x = post_norm_residual(x, hier_moe(modulate(x, sh2, sc2), *moe_w), g2, ln2_g[l], ln2_b[l])
    return x
```

```python
import math
import threading
from contextlib import ExitStack

import numpy as np
import concourse.bass as bass
import concourse.mybir as mybir
from concourse.bass_utils import run_bass_kernel_spmd

F32 = mybir.dt.float32
BF16 = mybir.dt.bfloat16
I32 = mybir.dt.int32
AF = mybir.ActivationFunctionType
ALU = mybir.AluOpType
AX = mybir.AxisListType

D = 1024
DPROJ = 3104
NE = 16
ALPHA = 2.0 ** 0.25
LN_EPS = 1e-5
RMS_EPS = 1e-6


class Buf:
    __slots__ = ("name", "w", "r", "dead", "psum")

    def __init__(self, name, psum=False):
        self.name = name
        self.w = None
        self.r = {}
        self.dead = False
        self.psum = psum

    def regen(self):
        n = Buf(self.name, self.psum)
        n.w = self.w
        n.r = dict(self.r)
        self.dead = True
        return n


class Sched:
    NAMES = ("pe", "act", "dve", "pool", "sp")
    ATTR = {"pe": "tensor", "act": "scalar", "dve": "vector", "pool": "gpsimd", "sp": "sync"}

    def __init__(self, nc, es, nds=32):
        self.nc = nc
        self.sem = {e: es.enter_context(nc.semaphore("s_" + e)) for e in self.NAMES}
        self.cnt = {e: 0 for e in self.NAMES}
        self.dsem = [es.enter_context(nc.semaphore("sd%d" % i)) for i in range(nds)]
        self.dval = [0] * nds
        self.dnext = 0
        self.dnext2 = 0
        self.ops = {e: [] for e in self.NAMES}
        self.waited = {e: {} for e in self.NAMES}

    def semof(self, k):
        return self.sem[k] if isinstance(k, str) else self.dsem[k]

    def _deps(self, eng, reads, writes):
        w = {}

        def add(ev):
            if ev is None:
                return
            k, v = ev
            if k == eng:
                if eng == "pe":
                    return
                if self.cnt[eng] - v >= 3:
                    return
            if w.get(k, 0) < v:
                w[k] = v

        for b in reads:
            assert not b.dead, "use of re-allocated buffer %s" % b.name
            add(b.w)
            if b.psum:
                for k, v in b.r.items():
                    if k != eng:
                        add((k, v))
        for b in writes:
            assert not b.dead, "use of re-allocated buffer %s" % b.name
            add(b.w)
            for k, v in b.r.items():
                add((k, v))
        out = []
        wd = self.waited[eng]
        for k, v in w.items():
            if wd.get(k, 0) >= v:
                continue
            wd[k] = v
            out.append((k, v))
        return out

    @staticmethod
    def _mark(ev, reads, writes):
        k, v = ev
        for b in reads:
            if b.r.get(k, 0) < v:
                b.r[k] = v
        for b in writes:
            b.w = ev
            b.r = {}

    def op(self, eng, fn, reads=(), writes=()):
        waits = self._deps(eng, reads, writes)
        self.cnt[eng] += 1
        ev = (eng, self.cnt[eng])
        self._mark(ev, reads, writes)
        self.ops[eng].append((waits, fn, ("inc", eng)))
        if Coro.cur is not None:
            Coro.cur.yield_()

    def dma(self, q, out_ap, in_ap, reads=(), writes=()):
        half = len(self.dsem) // 2
        if q == "sp":
            i = self.dnext
            self.dnext = (self.dnext + 1) % half
        else:
            i = half + self.dnext2
            self.dnext2 = (self.dnext2 + 1) % half
        waits = self._deps(q, reads, writes)
        prev = self.dval[i]
        if prev > 0 and self.waited[q].get(i, 0) < prev:
            self.waited[q][i] = prev
            waits.append((i, prev))
        self.dval[i] += 16
        ev = (i, self.dval[i])
        self._mark(ev, reads, writes)
        self.ops[q].append((waits, (lambda e, o=out_ap, s=in_ap: e.dma_start(out=o, in_=s)), ("dma", i)))
        if Coro.cur is not None:
            Coro.cur.yield_()

    def wait_all_dma(self, q):
        waits = []
        for i, v in enumerate(self.dval):
            if v > 0 and self.waited[q].get(i, 0) < v:
                self.waited[q][i] = v
                waits.append((i, v))
        self.ops[q].append((waits, None, None))

    def flush(self):
        nc = self.nc
        self.wait_all_dma("sp")
        with nc.Block() as blk:
            for e in self.NAMES:
                ops = self.ops[e]
                if not ops:
                    continue

                def body(eng, ops=ops):
                    for waits, fn, tag in ops:
                        for k, v in waits:
                            eng.wait_ge(self.semof(k), v)
                        if fn is None:
                            continue
                        ins = fn(eng)
                        if tag[0] == "inc":
                            ins.then_inc(self.sem[tag[1]], 1)
                        else:
                            ins.then_inc(self.dsem[tag[1]], 16)

                getattr(blk, self.ATTR[e])(body)
        self.ops = {e: [] for e in self.NAMES}


class _Stop(Exception):
    pass


class Coro:
    cur = None

    def __init__(self, fn):
        self.fn = fn
        self.done = False
        self.nops = 0
        self.exc = None
        self.go = threading.Semaphore(0)
        self.back = threading.Semaphore(0)
        self.t = threading.Thread(target=self._run, daemon=True)
        self.started = False

    def _run(self):
        self.go.acquire()
        try:
            self.fn()
        except BaseException as e:
            self.exc = e
        self.done = True
        self.back.release()

    def step(self):
        if self.done:
            return
        if not self.started:
            self.started = True
            self.t.start()
        prev = Coro.cur
        Coro.cur = self
        self.go.release()
        self.back.acquire()
        Coro.cur = prev
        if self.exc is not None:
            raise self.exc

    def yield_(self):
        self.nops += 1
        self.back.release()
        self.go.acquire()


def run_pipeline(bodies, max_active, lag):
    active = []
    i = 0
    while i < len(bodies) or active:
        if i < len(bodies) and len(active) < max_active and (not active or active[-1].nops >= lag):
            active.append(Coro(bodies[i]))
            i += 1
        for co in list(active):
            co.step()
            if co.done:
                active.remove(co)


def run_multi(items):
    cos = [(Coro(f), q) for f, q in items if f is not None]
    while any(not c.done for c, _ in cos):
        for c, q in cos:
            for _ in range(q):
                c.step()


def run_pair(fb, fa, qb=2, qa=1):
    cb = Coro(fb)
    ca = Coro(fa) if fa is not None else None
    while not cb.done or (ca is not None and not ca.done):
        for _ in range(qb):
            cb.step()
        if ca is not None:
            for _ in range(qa):
                ca.step()


class Ring:
    UID = 0

    def __init__(self, es, nc, name, shape, dtype, n):
        self.n = n
        Ring.UID += 1
        self.t = es.enter_context(nc.sbuf_tensor("%s_r%d" % (name, Ring.UID), [shape[0], n] + list(shape[1:]), dtype))
        self.bufs = [Buf("%s%d" % (name, i)) for i in range(n)]
        self.i = 0

    def next(self):
        i = self.i
        self.i = (self.i + 1) % self.n
        self.bufs[i] = self.bufs[i].regen()
        return self.bufs[i], self.t[:, i]


def build_program(SEQ, TSC, CTX=256, dbg=False, stop=None):
    NCH = SEQ // 128
    NST = NCH // TSC
    TS = TSC * 128
    TT = min(512, TS)
    NT = TS // TT
    NSUB = TT // 128
    NCC = CTX // 128

    nc = bass.Bass("TRN2", target_bir_lowering=False)

    def din(name, shape, dt=F32):
        return nc.dram_tensor(name, list(shape), dt, kind="ExternalInput").ap()

    x_d = din("x", [SEQ, D])
    ctx_d = din("ctx", [CTX, D])
    ccol_d = din("ccol", [128, 8, 2])
    wada_d = din("w_ada", [D, 6 * D])
    bada4_d = din("bada4", [128, 32])
    badarow_d = din("bada_row", [1, 6 * D])
    lncols_d = din("lncols", [128, 8, 4])
    lnrows_d = din("lnrows", [6, D])
    win_d = din("w_in", [D, DPROJ])
    convc_d = din("convcols", [128, 4, 4])
    gwaug_d = din("gw_aug", [33, 512])
    normg_d = din("normg_col", [128, 1])
    wout_d = din("w_out", [D, D])
    wr_d = din("wr", [D, 20])
    br_d = din("br_row", [1, 20])
    ew1_d = din("ew1", [NE, D, 512])
    ew3_d = din("ew3", [NE, D, 512])
    ew2_d = din("ew2", [NE, 512, D])
    ident_d = din("ident", [128, 128])
    texf_d = din("texf", [128, 128])
    texb_d = din("texb", [128, 128])
    tincf_d = din("tincf", [128, 128])
    tincb_d = din("tincb", [128, 128])
    mfp_d = din("mfp", [128, 512])
    mbs_d = din("mbs", [128, 512], I32)
    negcol_d = din("negcol", [128, 2])
    out_d = nc.dram_tensor("out", [SEQ, D], F32, kind="ExternalOutput").ap()
    x1a_d = nc.dram_tensor("x1a_scr", [SEQ, D], F32, kind="Internal").ap()
    sbscr_d = nc.dram_tensor("sb_scr", [NCH, 128, 2, 128], BF16, kind="Internal").ap()
    h2T_d = nc.dram_tensor("h2t_scr", [NCH, 128, 8, 128], BF16, kind="Internal").ap()
    comb_d = nc.dram_tensor("comb_scr", [NCH, 128, 16], F32, kind="Internal").ap()
    dbg_d = {}
    if dbg:
        for nm, shp in (("d_x1a", [SEQ, D]), ("d_sf", [128, 512]), ("d_sb", [128, 512]),
                        ("d_comb", [128, NCH, 16]), ("d_h2t", [128, 8, TS])):
            dbg_d[nm] = nc.dram_tensor(nm, shp, F32, kind="ExternalOutput").ap()

    es_all = ExitStack()
    try:
        _build_body(nc, es_all, locals())
    except _Stop:
        pass
    return nc


NPIPE, LAGP = 4, 12


def _build_body(nc, es_all, L):
    globals_ = L
    (SEQ, TSC, CTX, dbg, stop, NCH, NST, TS, TT, NT, NSUB, NCC) = (L[k] for k in ("SEQ", "TSC", "CTX", "dbg", "stop", "NCH", "NST", "TS", "TT", "NT", "NSUB", "NCC"))
    (x_d, ctx_d, ccol_d, wada_d, bada4_d, badarow_d, lncols_d, lnrows_d, win_d, convc_d, gwaug_d, normg_d, wout_d, wr_d, br_d,
     ew1_d, ew3_d, ew2_d, ident_d, texf_d, texb_d, tincf_d, tincb_d, mfp_d, mbs_d, negcol_d, out_d, x1a_d, dbg_d, sbscr_d, h2T_d, comb_d) = (
        L[k] for k in ("x_d", "ctx_d", "ccol_d", "wada_d", "bada4_d", "badarow_d", "lncols_d", "lnrows_d", "win_d", "convc_d", "gwaug_d",
                       "normg_d", "wout_d", "wr_d", "br_d", "ew1_d", "ew3_d", "ew2_d", "ident_d", "texf_d", "texb_d", "tincf_d", "tincb_d",
                       "mfp_d", "mbs_d", "negcol_d", "out_d", "x1a_d", "dbg_d", "sbscr_d", "h2T_d", "comb_d"))
    with es_all as es:
        S = Sched(nc, es)

        def chk(name):
            if stop == name:
                S.wait_all_dma("sp")
                S.flush()
                raise _Stop()

        uid = {"n": 0}

        def sb(name, shape, dt=F32, stack=es):
            uid["n"] += 1
            return stack.enter_context(nc.sbuf_tensor("%s_s%d" % (name, uid["n"]), list(shape), dt))

        ident = sb("ident", [128, 128]); B_ident = Buf("ident")
        identb = sb("identb", [128, 128], BF16)
        texf = sb("texf", [128, 128]); texb = sb("texb", [128, 128])
        tincf = sb("tincf", [128, 128]); tincb = sb("tincb", [128, 128])
        negcol = sb("negcol", [128, 2])
        B_const = Buf("consts")
        lncols = sb("lncols", [128, 8, 4])
        convc = sb("convc", [128, 4, 4])
        gwaug = sb("gwaug", [33, 512])
        normg = sb("normg", [128, 1])
        cols = sb("cols", [128, 6, 8])
        B_cols = Buf("cols")
        g2bc = sb("g2bc", [128, D]); B_gbc = Buf("gbc")
        wrp = sb("wrp", [128, 8, 20]); brp = sb("brp", [128, 20]); B_wrp = Buf("wrp")
        Sf = sb("Sf", [128, 512]); B_Sf = Buf("Sf")
        B_Sb = Buf("Sb")
        Sfb = Ring(es, nc, "Sfb", [128, 512], BF16, 2)
        B_sbpl = [Buf("sbpl%d" % c) for c in range(NCH)]
        B_h2d = [Buf("h2d%d" % c) for c in range(NCH)]
        B_cmbd = [Buf("cmbd%d" % c) for c in range(NCH)]
        B_x1ad = [Buf("x1ad%d" % c) for c in range(NCH)]
        win = sb("win", [128, 8, DPROJ], BF16)
        B_winl = [Buf("win%d" % i) for i in range(16)]
        wout = sb("wout", [128, 8, D], BF16); B_wout = Buf("wout")
        B_out = [Buf("out%d" % c) for c in range(NCH)]

        def ld_const(t, src, bufs):
            S.dma("sp", t[:], src, writes=bufs)

        with ExitStack() as bs:
            def lsb(name, shape, dt=F32):
                return sb(name, shape, dt, stack=bs)

            def ps(name):
                return bs.enter_context(nc.psum_tensor(name, [128, 1024], F32))

            PB = [ps("pb%d" % i) for i in range(4)]
            B_PB = [Buf("pbk%d" % i, True) for i in range(8)]
            pstate = {"i": 0}

            def bank():
                i = pstate["i"]
                pstate["i"] = (i + 1) % 8
                B_PB[i] = B_PB[i].regen()
                return B_PB[i], PB[i // 2][:, (i % 2) * 512:(i % 2) * 512 + 512]

            def bank2():
                if pstate["i"] % 2:
                    pstate["i"] = (pstate["i"] + 1) % 8
                i = pstate["i"]
                pstate["i"] = (i + 2) % 8
                B_PB[i] = B_PB[i].regen()
                B_PB[i + 1] = B_PB[i + 1].regen()
                return [B_PB[i], B_PB[i + 1]], PB[i // 2][:]

            ones = lsb("ones", [128, 128])
            g1bc = lsb("g1bc", [128, D])
            Sb = lsb("Sb", [128, 512])
            for t, src in ((ident, ident_d), (texf, texf_d), (texb, texb_d), (tincf, tincf_d),
                           (tincb, tincb_d), (negcol, negcol_d),
                           (lncols, lncols_d), (convc, convc_d), (gwaug, gwaug_d), (normg, normg_d)):
                ld_const(t, src, [B_const])
            for k in range(8):
                S.dma("pool", win[:, k, 0:1552], win_d[k * 128:(k + 1) * 128, 0:1552], writes=[B_winl[2 * k]])
                S.dma("pool", win[:, k, 1552:DPROJ], win_d[k * 128:(k + 1) * 128, 1552:DPROJ], writes=[B_winl[2 * k + 1]])
            S.dma("pool", wout[:], wout_d.rearrange("(k p) n -> p k n", p=128), writes=[B_wout])
            S.op("dve", lambda e: e.memset(ones[:], 1.0), writes=[B_const])
            S.op("dve", lambda e: e.tensor_copy(out=identb[:], in_=ident[:]), reads=[B_const], writes=[B_ident])
            S.op("dve", lambda e: e.memset(Sf[:], 0.0), writes=[B_Sf])
            S.op("dve", lambda e: e.memset(Sb[:], 0.0), writes=[B_Sb])

            ccol = lsb("ccol", [128, 8, 2]); B_cc = Buf("ccol")
            scs = lsb("scs", [128, 8, 2])
            scb = lsb("scb", [128, 8, 128]); B_scb = Buf("scb")
            bada4 = lsb("bada4", [128, 32])
            modc = lsb("modc", [128, 32, 2]); B_mod = Buf("mod")
            S.dma("sp", ccol[:], ccol_d, writes=[B_cc])
            S.dma("sp", bada4[:], bada4_d, writes=[B_cc])
            S.dma("sp", g1bc[:], badarow_d[0, 2 * D:3 * D].partition_broadcast(128), writes=[B_gbc])
            S.dma("sp", g2bc[:], badarow_d[0, 5 * D:6 * D].partition_broadcast(128), writes=[B_gbc])
            S.op("act", lambda e: e.activation(out=scs[:], in_=ccol[:], func=AF.Silu), reads=[B_cc], writes=[B_cc])
            for k in range(8):
                S.op("dve", lambda e, k=k: e.tensor_scalar(out=scb[:, k, :], in0=ones[:], scalar1=scs[:, k, 0:1],
                                                           scalar2=None, op0=ALU.mult),
                     reads=[B_cc, B_const], writes=[B_scb])
            slab = Ring(bs, nc, "slab", [128, 8, 512], F32, 2)
            B_pcol, pcol = bank()
            colkind = {0: 0, 1: 1, 3: 2, 4: 3}
            for s in range(12):
                kind, half = s // 2, s % 2
                B_sl, sl = slab.next()
                S.dma("sp", sl, wada_d[:, s * 512:(s + 1) * 512].rearrange("(k p) n -> p k n", p=128), writes=[B_sl])
                if kind in colkind:
                    for f in range(4):
                        idx = colkind[kind] * 8 + half * 4 + f

                        def fn(e, sl=sl, f=f, idx=idx):
                            for k in range(8):
                                ins = e.matmul(pcol[:, idx * 2:idx * 2 + 2], lhsT=sl[:, k, f * 128:(f + 1) * 128],
                                               rhs=scs[:, k, :], start=(k == 0), stop=(k == 7))
                            return ins
                        S.op("pe", fn, reads=[B_sl, B_cc], writes=[B_pcol])
                else:
                    B_pg, pg = bank()

                    def fn(e, sl=sl, pg=pg):
                        for k in range(8):
                            ins = e.matmul(pg, lhsT=scb[:, k, :], rhs=sl[:, k, :], start=(k == 0), stop=(k == 7))
                        return ins
                    S.op("pe", fn, reads=[B_sl, B_scb], writes=[B_pg])
                    gt = g1bc if kind == 2 else g2bc
                    S.op("dve", lambda e, gt=gt, pg=pg, half=half: e.tensor_tensor(
                        out=gt[:, half * 512:(half + 1) * 512], in0=pg, in1=gt[:, half * 512:(half + 1) * 512], op=ALU.add),
                        reads=[B_pg, B_gbc], writes=[B_gbc])
            for j in range(2):
                S.op("dve", lambda e, j=j: e.tensor_tensor(out=modc[:, :, j], in0=pcol[:, 0:64].rearrange("p (i t) -> p i t", t=2)[:, :, j], in1=bada4[:], op=ALU.add),
                     reads=[B_pcol, B_cc], writes=[B_mod])
            tmpc = lsb("tmpc", [128, 8])

            def derive(gi, bi, g_idx, b_idx, j, sh_off, sc_off):
                S.op("dve", lambda e: e.tensor_scalar(out=tmpc[:], in0=modc[:, sc_off:sc_off + 8, j], scalar1=1.0,
                                                      scalar2=None, op0=ALU.add), reads=[B_mod], writes=[B_mod])
                S.op("dve", lambda e: e.tensor_tensor(out=cols[:, gi, :], in0=lncols[:, :, g_idx], in1=tmpc[:], op=ALU.mult),
                     reads=[B_mod, B_const], writes=[B_cols])
                S.op("dve", lambda e: e.tensor_tensor(out=tmpc[:], in0=lncols[:, :, b_idx], in1=tmpc[:], op=ALU.mult),
                     reads=[B_mod, B_const], writes=[B_mod])
                S.op("dve", lambda e: e.tensor_tensor(out=cols[:, bi, :], in0=tmpc[:], in1=modc[:, sh_off:sh_off + 8, j], op=ALU.add),
                     reads=[B_mod], writes=[B_cols])

            derive(0, 1, 0, 1, 0, 0, 8)
            derive(2, 3, 0, 1, 1, 0, 8)
            derive(4, 5, 2, 3, 0, 16, 24)
            wrs = lsb("wrs", [128, 8, 20]); B_wrs = Buf("wrs")
            S.dma("sp", wrs[:], wr_d.rearrange("(k p) n -> p k n", p=128), writes=[B_wrs])
            S.dma("sp", brp[:], br_d[0].partition_broadcast(128), writes=[B_wrp])
            for k in range(8):
                S.op("dve", lambda e, k=k: e.tensor_scalar(out=wrp[:, k, :], in0=wrs[:, k, :], scalar1=cols[:, 4, k:k + 1],
                                                           scalar2=None, op0=ALU.mult),
                     reads=[B_wrs, B_cols], writes=[B_wrp])
                S.op("dve", lambda e, k=k: e.tensor_scalar(out=scb[:, k, :], in0=ones[:], scalar1=cols[:, 5, k:k + 1],
                                                           scalar2=None, op0=ALU.mult),
                     reads=[B_cols, B_const], writes=[B_scb])
            B_pb2, pb2 = bank()

            def fn(e):
                for k in range(8):
                    ins = e.matmul(pb2[:, 0:20], lhsT=scb[:, k, :], rhs=wrs[:, k, :], start=(k == 0), stop=(k == 7))
                return ins
            S.op("pe", fn, reads=[B_scb, B_wrs], writes=[B_pb2])
            S.op("dve", lambda e: e.tensor_tensor(out=brp[:], in0=pb2[:, 0:20], in1=brp[:], op=ALU.add),
                 reads=[B_pb2, B_wrp], writes=[B_wrp])

            for k in range(8):
                if k < 4:
                    S.op("dve", lambda e, k=k: e.tensor_tensor(out=wout[:, k, :], in0=wout[:, k, :], in1=g1bc[:], op=ALU.mult),
                         reads=[B_gbc, B_wout], writes=[B_wout])
                else:
                    S.op("dve", lambda e, k=k: e.scalar_tensor_tensor(out=wout[:, k, :], in0=wout[:, k, :], scalar=normg[:, 0:1], in1=g1bc[:],
                                                                      op0=ALU.mult, op1=ALU.mult),
                         reads=[B_gbc, B_wout, B_const], writes=[B_wout])
            if stop == "p0a":
                chk("p0a")

            xin = Ring(bs, nc, "xin", [128, D], F32, 4)
            xhat = Ring(bs, nc, "xhat", [128, D], F32, 4)
            hTr = Ring(bs, nc, "hT", [128, 8, 128], BF16, 4)
            stt = Ring(bs, nc, "stt", [128, 16], F32, 4)
            lowaug = Ring(bs, nc, "lowaug", [33, 128], F32, 4)
            e1r = Ring(bs, nc, "e1", [128, 512], F32, 4)
            Gr = Ring(bs, nc, "G", [128, 512], F32, 4)
            ekr = Ring(bs, nc, "ek", [128, 256], F32, 4)
            kendr = Ring(bs, nc, "kend", [128, 256], BF16, 4)
            vbr = Ring(bs, nc, "vb", [128, 512], BF16, 4)
            decr = Ring(bs, nc, "dec", [128, 4], F32, 4)
            sbstr = Ring(bs, nc, "sbst", [128, 2, 128], BF16, 4)
            for i in range(4):
                S.op("dve", lambda e, i=i: e.memset(lowaug.t[:, i], 1.0), writes=[lowaug.bufs[i]])

            class XLoader:
                def __init__(self, src_d, order):
                    self.src_d, self.order, self.q, self.nxt = src_d, list(order), {}, 0

                def get(self, c):
                    i = self.order.index(c)
                    depth = xin.n - 1
                    while self.nxt < len(self.order) and self.nxt <= i + depth:
                        cc = self.order[self.nxt]
                        B_x, xt = xin.next()
                        S.dma("sp", xt, self.src_d[cc * 128:(cc + 1) * 128, :], writes=[B_x])
                        self.q[cc] = (B_x, xt)
                        self.nxt += 1
                    return self.q.pop(c)

            def front(ldr, c, gi, bi, xh_ring=xhat, keep=None):
                B_x, xt = ldr.get(c)
                B_st, st = stt.next()
                for h in range(2):
                    S.op("dve", lambda e, h=h, st=st, xt=xt: e.bn_stats(out=st[:, h * 6:(h + 1) * 6], in_=xt[:, h * 512:(h + 1) * 512]),
                         reads=[B_x], writes=[B_st])
                S.op("dve", lambda e, st=st: e.bn_aggr(out=st[:, 12:14], in_=st[:, 0:12]), reads=[B_st], writes=[B_st])
                S.op("act", lambda e, st=st: e.activation(out=st[:, 14:15], in_=st[:, 13:14], func=AF.Ln, bias=LN_EPS, scale=1.0),
                     reads=[B_st], writes=[B_st])
                S.op("act", lambda e, st=st: e.activation(out=st[:, 14:15], in_=st[:, 14:15], func=AF.Exp, scale=-0.5),
                     reads=[B_st], writes=[B_st])
                S.op("dve", lambda e, st=st: e.scalar_tensor_tensor(out=st[:, 15:16], in0=st[:, 12:13], scalar=-1.0, in1=st[:, 14:15],
                                                                    op0=ALU.mult, op1=ALU.mult), reads=[B_st], writes=[B_st])
                B_xh, xh = xh_ring.next()
                S.op("act", lambda e, st=st, xt=xt, xh=xh: e.activation(out=xh, in_=xt, func=AF.Identity, bias=st[:, 15:16], scale=st[:, 14:15]),
                     reads=[B_st, B_x], writes=[B_xh])
                B_tp, tp = bank2()

                def fn(e, xh=xh, tp=tp):
                    for k in range(8):
                        ins = e.transpose(out=tp[:, k * 128:(k + 1) * 128], in_=xh[:, k * 128:(k + 1) * 128], identity=ident[:])
                    return ins
                S.op("pe", fn, reads=[B_xh, B_const], writes=B_tp)
                B_h, hT = hTr.next()
                for kk in range(8):
                    k = (kk // 2) + (4 if kk % 2 else 0)
                    if k < 4:
                        S.op("act", lambda e, k=k, hT=hT, tp=tp: e.activation(out=hT[:, k, :], in_=tp[:, k * 128:(k + 1) * 128], func=AF.Identity,
                                                                             bias=cols[:, bi, k:k + 1], scale=cols[:, gi, k:k + 1]),
                             reads=[B_tp[0], B_cols], writes=[B_h])
                    else:
                        S.op("dve", lambda e, k=k, hT=hT, tp=tp: e.tensor_scalar(out=hT[:, k, :], in0=tp[:, k * 128:(k + 1) * 128],
                                                                                scalar1=cols[:, gi, k:k + 1], scalar2=cols[:, bi, k:k + 1],
                                                                                op0=ALU.mult, op1=ALU.add),
                             reads=[B_tp[1], B_cols], writes=[B_h])
                return B_xh, xh, B_h, hT, B_tp, tp

            def gates(B_h, hT, wt, B_wt, goff):
                B_pl, pl = bank()

                def fn(e):
                    for k in range(8):
                        ins = e.matmul(pl[0:32, 0:128], lhsT=wt[:, k, goff:goff + 32], rhs=hT[:, k, :], start=(k == 0), stop=(k == 7))
                    return ins
                S.op("pe", fn, reads=[B_h] + B_wt, writes=[B_pl])
                B_la, la = lowaug.next()
                S.op("act", lambda e: e.activation(out=la[0:32, :], in_=pl[0:32, 0:128], func=AF.Copy), reads=[B_pl], writes=[B_la])
                B_pz, pz = bank()
                S.op("pe", lambda e: e.matmul(pz, lhsT=la[0:33, :], rhs=gwaug[:], start=True, stop=True),
                     reads=[B_la, B_const], writes=[B_pz])
                B_e1, e1 = e1r.next()
                S.op("act", lambda e: e.activation(out=e1, in_=pz, func=AF.Exp, scale=-1.0), reads=[B_pz], writes=[B_e1])
                B_G, G = Gr.next()
                S.op("act", lambda e: e.activation(out=G, in_=e1, func=AF.Ln, bias=1.0, scale=1.0), reads=[B_e1], writes=[B_G])
                return B_G, G

            def kend_and_decay(B_G, G, d, pk, B_pk):
                tex = texf if d == 0 else texb
                B_pe, pe_ = bank()
                S.op("pe", lambda e: e.matmul(pe_[:, 0:256], lhsT=tex[:], rhs=G[:, d * 256:(d + 1) * 256], start=True, stop=True),
                     reads=[B_G, B_const], writes=[B_pe])
                B_ek, ek = ekr.next()
                S.op("act", lambda e: e.activation(out=ek, in_=pe_[:, 0:256], func=AF.Exp), reads=[B_pe], writes=[B_ek])
                B_ke, ke = kendr.next()
                S.op("dve", lambda e: e.tensor_tensor(out=ke, in0=pk, in1=ek, op=ALU.mult), reads=[B_pk, B_ek], writes=[B_ke])
                B_pd, pd = bank()

                def fn(e):
                    for pt in range(2):
                        ins = e.matmul(pd[:, pt * 2:pt * 2 + 2], lhsT=G[:, d * 256 + pt * 128:d * 256 + (pt + 1) * 128], rhs=negcol[:],
                                       start=True, stop=True)
                    return ins
                S.op("pe", fn, reads=[B_G, B_const], writes=[B_pd])
                B_dc, dc = decr.next()
                S.op("act", lambda e: e.activation(out=dc, in_=pd[:, 0:4], func=AF.Exp), reads=[B_pd], writes=[B_dc])
                return B_ke, ke, B_dc, dc

            def state_update(St, B_St, B_ke, ke, B_vb, vb, B_dc, dc):
                B_pu, pu = bank()

                def fn(e):
                    for pt in range(2):
                        ins = e.matmul(pu[:, pt * 256:(pt + 1) * 256], lhsT=ke[:, pt * 128:(pt + 1) * 128], rhs=vb[:, pt * 256:(pt + 1) * 256],
                                       start=True, stop=True)
                    return ins
                S.op("pe", fn, reads=[B_ke, B_vb], writes=[B_pu])
                for pt in range(2):
                    for hf in range(2):
                        rs = slice(hf * 64, (hf + 1) * 64)
                        cs = slice(pt * 256 + hf * 128, pt * 256 + (hf + 1) * 128)
                        S.op("dve", lambda e, rs=rs, cs=cs, pt=pt: e.scalar_tensor_tensor(
                            out=St[rs, cs], in0=St[rs, cs], scalar=dc[rs, pt * 2:pt * 2 + 1], in1=pu[rs, cs], op0=ALU.mult, op1=ALU.add),
                            reads=[B_pu, B_dc, B_St], writes=[B_St])

            def state_pass(src_d, order, d, gi, bi, St, B_St, store):
                ldr = XLoader(src_d, order)
                run_pipeline([(lambda c=c: state_chunk(ldr, c, d, gi, bi, St, B_St, store)) for c in order], NPIPE, LAGP)

            def state_chunk(ldr, c, d, gi, bi, St, B_St, store):
                B_xh, xh, B_h, hT, _, _ = front(ldr, c, gi, bi)
                B_G, G = gates(B_h, hT, win, B_winl, 3072)
                tex = texf if d == 0 else texb
                B_pe, pe_ = bank()
                S.op("pe", lambda e: e.matmul(pe_[:, 0:256], lhsT=tex[:], rhs=G[:, d * 256:(d + 1) * 256], start=True, stop=True),
                     reads=[B_G, B_const], writes=[B_pe])
                B_ek, ek = ekr.next()
                S.op("act", lambda e: e.activation(out=ek, in_=pe_[:, 0:256], func=AF.Exp), reads=[B_pe], writes=[B_ek])
                B_pd, pd = bank()

                def fn(e):
                    for pt in range(2):
                        ins = e.matmul(pd[:, pt * 2:pt * 2 + 2], lhsT=G[:, d * 256 + pt * 128:d * 256 + (pt + 1) * 128], rhs=negcol[:],
                                       start=True, stop=True)
                    return ins
                S.op("pe", fn, reads=[B_G, B_const], writes=[B_pd])
                B_dc, dc = decr.next()
                S.op("act", lambda e: e.activation(out=dc, in_=pd[:, 0:4], func=AF.Exp), reads=[B_pd], writes=[B_dc])
                B_pk, pk = bank()

                def fn(e):
                    for k in range(8):
                        ins = e.matmul(pk[:, 0:256], lhsT=hT[:, k, :], rhs=win[:, k, 1792:2048], start=(k == 0), stop=(k == 7))
                    return ins
                S.op("pe", fn, reads=[B_h] + B_winl, writes=[B_pk])
                B_ke, ke = kendr.next()
                S.op("dve", lambda e: e.tensor_tensor(out=ke, in0=pk[:, 0:256], in1=ek, op=ALU.mult), reads=[B_pk, B_ek], writes=[B_ke])
                B_pv, pv = bank()

                def fn(e):
                    for k in range(8):
                        ins = e.matmul(pv, lhsT=hT[:, k, :], rhs=win[:, k, 2048:2560], start=(k == 0), stop=(k == 7))
                    return ins
                S.op("pe", fn, reads=[B_h] + B_winl, writes=[B_pv])
                B_vb, vb = vbr.next()
                S.op("act", lambda e: e.activation(out=vb, in_=pv, func=AF.Copy), reads=[B_pv], writes=[B_vb])
                if store:
                    B_sbt, sbt = sbstr.next()
                    for pt in range(2):
                        for hf in range(2):
                            rs = slice(hf * 64, (hf + 1) * 64)
                            cs = slice(pt * 256 + hf * 128, pt * 256 + (hf + 1) * 128)
                            S.op("pool", lambda e, rs=rs, cs=cs, pt=pt: e.tensor_copy(out=sbt[rs, pt, :], in_=St[rs, cs]),
                                 reads=[B_St], writes=[B_sbt])
                    S.dma("sp", sbscr_d[c], sbt, reads=[B_sbt], writes=[B_sbpl[c]])
                state_update(St, B_St, B_ke, ke, B_vb, vb, B_dc, dc)

            state_pass(ctx_d, list(range(NCC)), 0, 2, 3, Sf, B_Sf, False)
            state_pass(ctx_d, list(range(NCC - 1, -1, -1)), 1, 2, 3, Sb, B_Sb, False)
            state_pass(x_d, list(range(NCH - 1, -1, -1)), 1, 0, 1, Sb, B_Sb, True)
            B_sf0, sf0 = Sfb.next()
            S.op("act", lambda e: e.activation(out=sf0, in_=Sf[:], func=AF.Copy), reads=[B_Sf], writes=[B_sf0])
            if dbg:
                S.dma("sp", dbg_d["d_sf"], Sf[:], reads=[B_Sf])
                S.dma("sp", dbg_d["d_sb"], Sb[:], reads=[B_Sb])
            if stop == "p0":
                S.wait_all_dma("sp")
            S.flush()
            if stop == "p0":
                raise _Stop()
        cur_sfb = {"B": B_sf0, "ap": sf0}

        for st_i in range(1):
            c0 = 0
            if st_i == 0:
                cm0, TSCM = 0, NCH
                with ExitStack() as bs:
                    def lsb(name, shape, dt=F32):
                        return sb(name, shape, dt, stack=bs)

                    PB = [bs.enter_context(nc.psum_tensor("pm%d_%d" % (st_i, i), [128, 1024], F32)) for i in range(4)]
                    B_PB = [Buf("pmk%d" % i, True) for i in range(8)]
                    pstate = {"A": 0, "B": 0}

                    def _pool():
                        return getattr(threading.current_thread(), "pool", "A")

                    def bank():
                        p = _pool()
                        j = pstate[p]
                        pstate[p] = (j + 1) % 4
                        i = j + (4 if p == "B" else 0)
                        B_PB[i] = B_PB[i].regen()
                        return B_PB[i], PB[i // 2][:, (i % 2) * 512:(i % 2) * 512 + 512]

                    def bank2():
                        p = _pool()
                        if pstate[p] % 2:
                            pstate[p] = (pstate[p] + 1) % 4
                        j = pstate[p]
                        pstate[p] = (j + 2) % 4
                        i = j + (4 if p == "B" else 0)
                        B_PB[i] = B_PB[i].regen()
                        B_PB[i + 1] = B_PB[i + 1].regen()
                        return [B_PB[i], B_PB[i + 1]], PB[i // 2][:]

                    sbpl = lsb("sbpl", [128, TSCM, 2, 128], BF16)
                    B_sbl = [Buf("sbl%d" % i) for i in range(TSCM)]
                    for i in range(TSCM):
                        S.dma("sp", sbpl[:, i], sbscr_d[cm0 + i], reads=[B_sbpl[cm0 + i]], writes=[B_sbl[i]])
                    mfp = lsb("mfp", [128, 512]); mbs = lsb("mbs", [128, 512], I32); B_msk = Buf("masks")
                    S.dma("sp", mfp[:], mfp_d, writes=[B_msk])
                    S.dma("sp", mbs[:], mbs_d, writes=[B_msk])
                    bct = lsb("bct", [128, 4, D]); B_bct = Buf("bct")
                    for i, r in enumerate((0, 1, 2, 3)):
                        S.dma("sp", bct[:, i, :], lnrows_d[r].partition_broadcast(128), writes=[B_bct])
                    S.op("act", lambda e: e.activation(out=bct[:], in_=bct[:], func=AF.Copy, scale=ALPHA), reads=[B_bct], writes=[B_bct])

                    xin = Ring(bs, nc, "xin", [128, D], F32, 2)
                    xhat = Ring(bs, nc, "xhat", [128, D], F32, 2)
                    hTr = Ring(bs, nc, "hT", [128, 8, 128], BF16, 2)
                    stt = Ring(bs, nc, "stt", [128, 16], F32, 4)
                    lowaug = Ring(bs, nc, "lowaug", [33, 128], F32, 2)
                    e1r = Ring(bs, nc, "e1", [128, 512], F32, 1)
                    Gr = Ring(bs, nc, "G", [128, 512], F32, 2)
                    ekr = Ring(bs, nc, "ek", [128, 256], F32, 1)
                    kendr = Ring(bs, nc, "kend", [128, 256], BF16, 2)
                    vbr = Ring(bs, nc, "vb", [128, 512], BF16, 2)
                    decr = Ring(bs, nc, "dec", [128, 4], F32, 2)
                    axr = Ring(bs, nc, "ax", [128, 512], F32, 1)
                    ur = Ring(bs, nc, "u", [128, 512], F32, 1)
                    cvr = Ring(bs, nc, "cv", [128, 512], F32, 1)
                    yar = Ring(bs, nc, "yaT", [128, 4, 128], BF16, 2)
                    qkr = Ring(bs, nc, "qk", [128, 4, 128], F32, 2)
                    ktr = Ring(bs, nc, "ktm", [128, 256], F32, 2)
                    srr = Ring(bs, nc, "sr", [128, 512], F32, 2)
                    eqr = Ring(bs, nc, "eq", [128, 4, 128], F32, 1)
                    ekk = Ring(bs, nc, "ekk", [128, 4, 128], F32, 1)
                    qdr = Ring(bs, nc, "qd", [128, 4, 128], BF16, 1)
                    kir = Ring(bs, nc, "ki", [128, 4, 128], BF16, 1)
                    qbd = lsb("qbd", [128, 4, 256], BF16); B_qbd = Buf("qbd")
                    sbd = lsb("sbd", [128, 512], BF16); B_sbd = Buf("sbd")
                    atr = Ring(bs, nc, "AT", [128, 512], BF16, 1)
                    ssr = Ring(bs, nc, "ss", [128, 16], F32, 2)
                    junk = lsb("junk", [128, 128]); B_junk = Buf("junk")
                    ybr = Ring(bs, nc, "yb", [128, 512], BF16, 1)
                    ybTr = Ring(bs, nc, "ybT", [128, 4, 128], BF16, 1)
                    x1hr = Ring(bs, nc, "x1h", [128, D], F32, 1)
                    x1Tr = Ring(bs, nc, "x1T", [128, D], F32, 1)
                    rtr = Ring(bs, nc, "rt", [128, 64], F32, 2)
                    h2r = Ring(bs, nc, "h2t", [128, 8, 128], BF16, 2)
                    cmbr = Ring(bs, nc, "cmb", [128, 16], F32, 3)
                    for i in range(2):
                        S.op("dve", lambda e, i=i: e.memset(lowaug.t[:, i], 1.0), writes=[lowaug.bufs[i]])
                    S.op("dve", lambda e: e.memset(qbd[:], 0.0), writes=[B_qbd])
                    S.op("dve", lambda e: e.memset(sbd[:], 0.0), writes=[B_sbd])

                    stash = {}
                    tails = {}

                    def stageA(c):
                        threading.current_thread().pool = "A"
                        cl = c - cm0
                        B_xh, xh, B_h, hT, _, _ = front(mldr, c, 0, 1, xh_ring=xhat)
                        B_G, G = gates(B_h, hT, win, B_winl, 3072)
                        B_pq, pq = bank()

                        def fn(e, hT=hT, pq=pq):
                            for g in range(4):
                                for k in range(8):
                                    ins = e.matmul(pq[:, g * 128:(g + 1) * 128], lhsT=win[:, k, 1536 + g * 128:1536 + (g + 1) * 128], rhs=hT[:, k, :],
                                                   start=(k == 0), stop=(k == 7))
                            return ins
                        S.op("pe", fn, reads=[B_h] + B_winl, writes=[B_pq])
                        B_qk, qk = qkr.next()
                        S.op("act", lambda e, qk=qk, pq=pq: e.activation(out=qk.rearrange("p a b -> p (a b)"), in_=pq, func=AF.Copy), reads=[B_pq], writes=[B_qk])
                        B_pk, pk = bank()
                        B_pv, pv = bank()
                        B_pr, pr = bank()

                        def fn(e, hT=hT, pk=pk, pv=pv, pr=pr):
                            for k in range(8):
                                e.matmul(pk[:, 0:256], lhsT=hT[:, k, :], rhs=win[:, k, 1792:2048], start=(k == 0), stop=(k == 7))
                            for k in range(8):
                                e.matmul(pv, lhsT=hT[:, k, :], rhs=win[:, k, 2048:2560], start=(k == 0), stop=(k == 7))
                            for k in range(8):
                                ins = e.matmul(pr, lhsT=hT[:, k, :], rhs=win[:, k, 2560:3072], start=(k == 0), stop=(k == 7))
                            return ins
                        S.op("pe", fn, reads=[B_h] + B_winl, writes=[B_pk, B_pv, B_pr])
                        B_kt, kt = ktr.next()
                        S.op("act", lambda e, kt=kt, pk=pk: e.activation(out=kt, in_=pk[:, 0:256], func=AF.Copy), reads=[B_pk], writes=[B_kt])
                        B_vb, vb = vbr.next()
                        S.op("act", lambda e, vb=vb, pv=pv: e.activation(out=vb, in_=pv, func=AF.Copy), reads=[B_pv], writes=[B_vb])
                        B_sr, sr = srr.next()
                        S.op("act", lambda e, sr=sr, pr=pr: e.activation(out=sr, in_=pr, func=AF.Silu), reads=[B_pr], writes=[B_sr])
                        B_ya, ya = yar.next()
                        B_pcc, pcc = bank()
                        B_pcx, pcx = bank()
                        B_pcb, pcb = bank()

                        def fn(e, hT=hT, pcc=pcc, pcx=pcx, pcb=pcb):
                            for off, pbk in ((512, pcc), (1024, pcx), (0, pcb)):
                                for j in range(4):
                                    for k in range(8):
                                        ins = e.matmul(pbk[:, j * 128:(j + 1) * 128], lhsT=win[:, k, off + j * 128:off + (j + 1) * 128], rhs=hT[:, k, :],
                                                       start=(k == 0), stop=(k == 7))
                            return ins
                        S.op("pe", fn, reads=[B_h] + B_winl, writes=[B_pcc, B_pcx, B_pcb])
                        B_ax, ax = axr.next()
                        S.op("act", lambda e, ax=ax, pcx=pcx: e.activation(out=ax, in_=pcx, func=AF.Copy), reads=[B_pcx], writes=[B_ax])
                        B_u, u = ur.next()
                        S.op("dve", lambda e, u=u, ax=ax, pcc=pcc: e.tensor_tensor(out=u, in0=pcc, in1=ax, op=ALU.mult),
                             reads=[B_pcc, B_ax], writes=[B_u])
                        B_cv, cv = cvr.next()
                        for j in range(4):
                            S.op("act", lambda e, cv=cv, u=u, j=j: e.activation(out=cv[:, j * 128:(j + 1) * 128], in_=u[:, j * 128:(j + 1) * 128], func=AF.Identity,
                                                                             bias=convc[:, j, 3:4], scale=convc[:, j, 1:2]),
                                 reads=[B_u, B_const], writes=[B_cv])
                        for j in range(4):
                            u3 = u[:, j * 128:(j + 1) * 128].rearrange("p (r w) -> p r w", w=64)
                            cv3 = cv[:, j * 128:(j + 1) * 128].rearrange("p (r w) -> p r w", w=64)
                            S.op("dve", lambda e, u3=u3, cv3=cv3, j=j: e.scalar_tensor_tensor(out=cv3[:, :, 1:64], in0=u3[:, :, 0:63], scalar=convc[:, j, 0:1],
                                                                                             in1=cv3[:, :, 1:64], op0=ALU.mult, op1=ALU.add),
                                 reads=[B_u, B_cv, B_const], writes=[B_cv])
                            S.op("dve", lambda e, u3=u3, cv3=cv3, j=j: e.scalar_tensor_tensor(out=cv3[:, :, 0:63], in0=u3[:, :, 1:64], scalar=convc[:, j, 2:3],
                                                                                             in1=cv3[:, :, 0:63], op0=ALU.mult, op1=ALU.add),
                                 reads=[B_u, B_cv, B_const], writes=[B_cv])
                        S.op("dve", lambda e, ya=ya, cv=cv, pcb=pcb: e.tensor_tensor(out=ya.rearrange("p a b -> p (a b)"), in0=pcb, in1=cv, op=ALU.mult),
                             reads=[B_pcb, B_cv], writes=[B_ya])
                        chk("a1")
                        chk("a2")
                        S.op("pool", lambda e, xh=xh: e.tensor_tensor(out=xh, in0=xh, in1=bct[:, 0, :], op=ALU.mult), reads=[B_bct], writes=[B_xh])
                        S.op("pool", lambda e, xh=xh: e.tensor_tensor(out=xh, in0=xh, in1=bct[:, 1, :], op=ALU.add), reads=[B_bct], writes=[B_xh])
                        chk("a3")
                        stash[c] = dict(B_xh=B_xh, xh=xh, B_ya=B_ya, ya=ya, B_qk=B_qk, qk=qk, B_kt=B_kt, kt=kt, B_vb=B_vb, vb=vb,
                                        B_sr=B_sr, sr=sr, B_G=B_G, G=G)

                    def stageB(c):
                        threading.current_thread().pool = "B"
                        cl = c - cm0
                        s = stash.pop(c)
                        B_G, G, qk, B_qk, vb, B_vb = s["B_G"], s["G"], s["qk"], s["B_qk"], s["vb"], s["B_vb"]
                        for pt in range(2):
                            for hf in range(2):
                                rs = slice(hf * 64, (hf + 1) * 64)
                                cs = slice(pt * 256 + hf * 128, pt * 256 + (hf + 1) * 128)
                                S.op("pool", lambda e, rs=rs, cs=cs, pt=pt: e.tensor_copy(out=sbd[rs, cs], in_=sbpl[rs, cl, pt, :]),
                                     reads=[B_sbl[cl]], writes=[B_sbd])
                        B_pg, pg = bank()

                        def fn(e):
                            for d in range(2):
                                tinc = tincf if d == 0 else tincb
                                for pt in range(2):
                                    i = d * 2 + pt
                                    ins = e.matmul(pg[:, i * 128:(i + 1) * 128], lhsT=G[:, d * 256 + pt * 128:d * 256 + (pt + 1) * 128], rhs=tinc[:],
                                                   start=True, stop=True)
                            return ins
                        S.op("pe", fn, reads=[B_G, B_const], writes=[B_pg])
                        B_eq, eq = eqr.next()
                        B_ek2, ek2 = ekk.next()
                        S.op("act", lambda e: e.activation(out=eq.rearrange("p a b -> p (a b)"), in_=pg, func=AF.Exp, bias=math.log(0.125), scale=1.0),
                             reads=[B_pg], writes=[B_eq])
                        S.op("act", lambda e: e.activation(out=ek2.rearrange("p a b -> p (a b)"), in_=pg, func=AF.Exp, scale=-1.0), reads=[B_pg], writes=[B_ek2])
                        B_qd, qd = qdr.next()
                        B_ki, ki = kir.next()
                        for d in range(2):
                            S.op("dve", lambda e, d=d: e.tensor_tensor(out=qd[:, d * 2:d * 2 + 2, :], in0=qk[:, 0:2, :], in1=eq[:, d * 2:d * 2 + 2, :], op=ALU.mult),
                                 reads=[B_qk, B_eq], writes=[B_qd])
                            S.op("dve", lambda e, d=d: e.tensor_tensor(out=ki[:, d * 2:d * 2 + 2, :], in0=qk[:, 2:4, :], in1=ek2[:, d * 2:d * 2 + 2, :], op=ALU.mult),
                                 reads=[B_qk, B_ek2], writes=[B_ki])
                        for hf in range(2):
                            rs = slice(hf * 64, (hf + 1) * 64)
                            S.op("act", lambda e, rs=rs, hf=hf: e.activation(out=qbd[rs, :, hf * 128:(hf + 1) * 128], in_=qd[rs, :, :], func=AF.Copy),
                                 reads=[B_qd], writes=[B_qbd])
                        chk("b1")
                        pAs = []
                        for d in range(2):
                            B_pa, pa = bank()

                            def fn(e, d=d, pa=pa):
                                for pt in range(2):
                                    i = d * 2 + pt
                                    ins = e.matmul(pa[:, pt * 256:(pt + 1) * 256], lhsT=ki[:, i, :], rhs=qbd[:, i, :], start=True, stop=True)
                                return ins
                            S.op("pe", fn, reads=[B_ki, B_qbd], writes=[B_pa])
                            pAs.append((B_pa, pa))
                        B_at, at = atr.next()
                        S.op("dve", lambda e: e.tensor_tensor(out=at, in0=pAs[0][1], in1=mfp[:], op=ALU.mult), reads=[pAs[0][0], B_msk], writes=[B_at])
                        S.op("dve", lambda e: e.copy_predicated(out=at, mask=mbs[:], data=pAs[1][1]), reads=[pAs[1][0], B_msk, B_at], writes=[B_at])
                        chk("b2")
                        B_sfc, sfc = cur_sfb["B"], cur_sfb["ap"]
                        B_po, po = bank()

                        def fn(e):
                            for h in range(4):
                                e.matmul(po[:, h * 128:(h + 1) * 128], lhsT=at[:, h * 128:(h + 1) * 128], rhs=vb[:, h * 128:(h + 1) * 128],
                                         start=(h == 0), stop=False, skip_group_check=True)
                            for pt in range(2):
                                e.matmul(po[:, pt * 256:(pt + 1) * 256], lhsT=qd[:, pt, :], rhs=sfc[:, pt * 256:(pt + 1) * 256],
                                         start=False, stop=False, skip_group_check=True)
                            for pt in range(2):
                                ins = e.matmul(po[:, pt * 256:(pt + 1) * 256], lhsT=qd[:, 2 + pt, :], rhs=sbd[:, pt * 256:(pt + 1) * 256],
                                               start=False, stop=(pt == 1), skip_group_check=True)
                            return ins
                        S.op("pe", fn, reads=[B_at, B_vb, B_qd, B_sfc, B_sbd], writes=[B_po])
                        chk("b3")
                        B_ke, ke, B_dc, dc = kend_and_decay(B_G, G, 0, s["kt"], s["B_kt"])
                        state_update(Sf, B_Sf, B_ke, ke, B_vb, vb, B_dc, dc)
                        B_sfn, sfn = Sfb.next()
                        S.op("act", lambda e: e.activation(out=sfn, in_=Sf[:], func=AF.Copy), reads=[B_Sf], writes=[B_sfn])
                        cur_sfb["B"], cur_sfb["ap"] = B_sfn, sfn
                        chk("b4")
                        B_ss, ss = ssr.next()
                        for h in range(4):
                            S.op("act", lambda e, h=h: e.activation(out=junk[:], in_=po[:, h * 128:(h + 1) * 128], func=AF.Square, accum_out=ss[:, h:h + 1]),
                                 reads=[B_po], writes=[B_junk, B_ss])
                        S.op("act", lambda e: e.activation(out=ss[:, 4:8], in_=ss[:, 0:4], func=AF.Ln, bias=RMS_EPS, scale=1.0 / 128.0), reads=[B_ss], writes=[B_ss])
                        S.op("act", lambda e: e.activation(out=ss[:, 8:12], in_=ss[:, 4:8], func=AF.Exp, scale=-0.5), reads=[B_ss], writes=[B_ss])
                        B_yb, yb = ybr.next()
                        sr = s["sr"]
                        for h in range(4):
                            S.op("dve", lambda e, h=h: e.scalar_tensor_tensor(out=yb[:, h * 128:(h + 1) * 128], in0=po[:, h * 128:(h + 1) * 128],
                                                                              scalar=ss[:, 8 + h:9 + h], in1=sr[:, h * 128:(h + 1) * 128],
                                                                              op0=ALU.mult, op1=ALU.mult),
                                 reads=[B_po, B_ss, s["B_sr"]], writes=[B_yb])
                        B_pt, pt_ = bank()
                        ptb = pt_.bitcast(BF16)

                        def fn(e):
                            for h in range(4):
                                ins = e.transpose(out=ptb[:, h * 128:(h + 1) * 128], in_=yb[:, h * 128:(h + 1) * 128], identity=identb[:])
                            return ins
                        S.op("pe", fn, reads=[B_yb, B_ident], writes=[B_pt])
                        B_ybT, ybT = ybTr.next()
                        S.op("act", lambda e: e.activation(out=ybT.rearrange("p a b -> p (a b)"), in_=ptb[:, 0:512], func=AF.Copy), reads=[B_pt], writes=[B_ybT])
                        chk("b5")
                        B_py, py = bank2()
                        ya = s["ya"]

                        def fn(e):
                            for hf in range(2):
                                for k in range(8):
                                    lhsT = ya[:, k, :] if k < 4 else ybT[:, k - 4, :]
                                    ins = e.matmul(py[:, hf * 512:(hf + 1) * 512], lhsT=lhsT, rhs=wout[:, k, hf * 512:(hf + 1) * 512],
                                                   start=(k == 0), stop=(k == 7))
                            return ins
                        S.op("pe", fn, reads=[s["B_ya"], B_ybT, B_wout], writes=B_py)
                        B_z1, z1 = s["B_xh"], s["xh"]
                        S.op("dve", lambda e: e.tensor_tensor(out=z1, in0=py, in1=s["xh"], op=ALU.add), reads=B_py + [s["B_xh"]], writes=[B_z1])
                        B_st, st = stt.next()
                        for h in range(2):
                            S.op("dve", lambda e, h=h: e.bn_stats(out=st[:, h * 6:(h + 1) * 6], in_=z1[:, h * 512:(h + 1) * 512]), reads=[B_z1], writes=[B_st])
                        S.op("dve", lambda e: e.bn_aggr(out=st[:, 12:14], in_=st[:, 0:12]), reads=[B_st], writes=[B_st])
                        S.op("act", lambda e: e.activation(out=st[:, 14:15], in_=st[:, 13:14], func=AF.Ln, bias=LN_EPS, scale=1.0), reads=[B_st], writes=[B_st])
                        S.op("act", lambda e: e.activation(out=st[:, 14:15], in_=st[:, 14:15], func=AF.Exp, scale=-0.5), reads=[B_st], writes=[B_st])
                        S.op("dve", lambda e: e.scalar_tensor_tensor(out=st[:, 15:16], in0=st[:, 12:13], scalar=-1.0, in1=st[:, 14:15],
                                                                     op0=ALU.mult, op1=ALU.mult), reads=[B_st], writes=[B_st])
                        B_x1h, x1h = x1hr.next()
                        S.op("act", lambda e: e.activation(out=x1h, in_=z1, func=AF.Identity, bias=st[:, 15:16], scale=st[:, 14:15]),
                             reads=[B_st, B_z1], writes=[B_x1h])
                        chk("b6")
                        B_tp, tp = bank2()

                        def fn(e):
                            for k in range(8):
                                ins = e.transpose(out=tp[:, k * 128:(k + 1) * 128], in_=x1h[:, k * 128:(k + 1) * 128], identity=ident[:])
                            return ins
                        S.op("pe", fn, reads=[B_x1h, B_const], writes=B_tp)
                        S.op("pool", lambda e: e.tensor_tensor(out=x1h, in0=x1h, in1=bct[:, 2, :], op=ALU.mult), reads=[B_bct], writes=[B_x1h])
                        S.op("pool", lambda e: e.tensor_tensor(out=x1h, in0=x1h, in1=bct[:, 3, :], op=ALU.add), reads=[B_bct], writes=[B_x1h])
                        chk("b6a")
                        S.dma("sp", x1a_d[c * 128:(c + 1) * 128, :], x1h, reads=[B_x1h], writes=[B_x1ad[c]])
                        chk("b6b")
                        if dbg:
                            S.dma("sp", dbg_d["d_x1a"][c * 128:(c + 1) * 128, :], x1h, reads=[B_x1h])
                        chk("b6d")
                        B_x1T, x1T = x1Tr.next()
                        S.op("act", lambda e: e.activation(out=x1T[:, 0:512], in_=tp[:, 0:512], func=AF.Copy), reads=[B_tp[0]], writes=[B_x1T])
                        S.op("dve", lambda e: e.tensor_copy(out=x1T[:, 512:1024], in_=tp[:, 512:1024]), reads=[B_tp[1]], writes=[B_x1T])
                        chk("b6c")
                        B_prt, prt = bank()

                        def fn(e):
                            for k in range(8):
                                ins = e.matmul(prt[:, 0:20], lhsT=x1T[:, k * 128:(k + 1) * 128], rhs=wrp[:, k, :], start=(k == 0), stop=(k == 7))
                            return ins
                        S.op("pe", fn, reads=[B_x1T, B_wrp], writes=[B_prt])
                        chk("b7")
                        B_rt, rt = rtr.next()
                        S.op("dve", lambda e: e.tensor_tensor(out=rt[:, 0:20], in0=prt[:, 0:20], in1=brp[:], op=ALU.add), reads=[B_prt, B_wrp], writes=[B_rt])
                        B_h2t, h2t = h2r.next()
                        for kk in range(8):
                            k = (kk // 2) + (4 if kk % 2 else 0)
                            if k < 4:
                                S.op("act", lambda e, k=k: e.activation(out=h2t[:, k, :], in_=tp[:, k * 128:(k + 1) * 128], func=AF.Identity,
                                                                        bias=cols[:, 5, k:k + 1], scale=cols[:, 4, k:k + 1]),
                                     reads=[B_tp[0], B_cols], writes=[B_h2t])
                            else:
                                S.op("dve", lambda e, k=k: e.tensor_scalar(out=h2t[:, k, :], in0=tp[:, k * 128:(k + 1) * 128],
                                                                           scalar1=cols[:, 4, k:k + 1], scalar2=cols[:, 5, k:k + 1],
                                                                           op0=ALU.mult, op1=ALU.add),
                                     reads=[B_tp[1], B_cols], writes=[B_h2t])
                        S.dma("sp", h2T_d[c], h2t, reads=[B_h2t], writes=[B_h2d[c]])
                        tails[c] = lambda: route_tail(c, cl, B_rt, rt)

                    def route_tail(c, cl, B_rt, rt):
                        B_cmb, cmb = cmbr.next()
                        R = [B_rt]
                        lg = rt[:, 0:20]
                        mg = rt[:, 20:21]; eg = rt[:, 21:25]; sg = rt[:, 25:26]; pgp = rt[:, 26:27]; oh = rt[:, 27:31]
                        sel = rt[:, 31:35]; m2 = rt[:, 35:36]; es_ = rt[:, 36:40]; mk1 = rt[:, 40:44]; esm = rt[:, 44:48]
                        e2 = rt[:, 48:49]; mk2 = rt[:, 49:53]; den = rt[:, 53:54]; wi = rt[:, 54:58]; ohp = rt[:, 58:62]; nm = rt[:, 62:64]
                        S.op("dve", lambda e: e.tensor_reduce(out=mg, in_=rt[:, 0:4], axis=AX.X, op=ALU.max), reads=R, writes=R)
                        S.op("dve", lambda e: e.tensor_scalar(out=nm[:, 0:1], in0=mg, scalar1=-1.0, scalar2=None, op0=ALU.mult), reads=R, writes=R)
                        S.op("act", lambda e: e.activation(out=eg, in_=rt[:, 0:4], func=AF.Exp, bias=nm[:, 0:1], scale=1.0, accum_out=sg), reads=R, writes=R)
                        S.op("dve", lambda e: e.reciprocal(out=pgp, in_=sg), reads=R, writes=R)
                        S.op("dve", lambda e: e.tensor_scalar(out=oh, in0=rt[:, 0:4], scalar1=mg, scalar2=None, op0=ALU.is_equal), reads=R, writes=R)
                        S.op("dve", lambda e: e.tensor_scalar(out=sel, in0=rt[:, 4:8], scalar1=oh[:, 0:1], scalar2=None, op0=ALU.mult), reads=R, writes=R)
                        for g in range(1, 4):
                            S.op("dve", lambda e, g=g: e.scalar_tensor_tensor(out=sel, in0=rt[:, 4 + 4 * g:8 + 4 * g], scalar=oh[:, g:g + 1], in1=sel,
                                                                              op0=ALU.mult, op1=ALU.add), reads=R, writes=R)
                        S.op("dve", lambda e: e.tensor_reduce(out=m2, in_=sel, axis=AX.X, op=ALU.max), reads=R, writes=R)
                        S.op("dve", lambda e: e.tensor_scalar(out=nm[:, 1:2], in0=m2, scalar1=-1.0, scalar2=None, op0=ALU.mult), reads=R, writes=R)
                        S.op("act", lambda e: e.activation(out=es_, in_=sel, func=AF.Exp, bias=nm[:, 1:2], scale=1.0), reads=R, writes=R)
                        S.op("dve", lambda e: e.tensor_scalar(out=mk1, in0=sel, scalar1=m2, scalar2=None, op0=ALU.is_equal), reads=R, writes=R)
                        S.op("dve", lambda e: e.scalar_tensor_tensor(out=esm, in0=mk1, scalar=-2.0, in1=es_, op0=ALU.mult, op1=ALU.add), reads=R, writes=R)
                        S.op("dve", lambda e: e.tensor_reduce(out=e2, in_=esm, axis=AX.X, op=ALU.max), reads=R, writes=R)
                        S.op("dve", lambda e: e.tensor_scalar(out=mk2, in0=esm, scalar1=e2, scalar2=None, op0=ALU.is_equal), reads=R, writes=R)
                        S.op("dve", lambda e: e.tensor_scalar(out=den, in0=e2, scalar1=1.0, scalar2=None, op0=ALU.add), reads=R, writes=R)
                        S.op("dve", lambda e: e.reciprocal(out=den, in_=den), reads=R, writes=R)
                        S.op("dve", lambda e: e.tensor_tensor(out=wi, in0=mk1, in1=mk2, op=ALU.add), reads=R, writes=R)
                        S.op("dve", lambda e: e.tensor_tensor(out=wi, in0=wi, in1=es_, op=ALU.mult), reads=R, writes=R)
                        S.op("dve", lambda e: e.tensor_scalar(out=wi, in0=wi, scalar1=den, scalar2=None, op0=ALU.mult), reads=R, writes=R)
                        S.op("dve", lambda e: e.tensor_scalar(out=ohp, in0=oh, scalar1=pgp, scalar2=None, op0=ALU.mult), reads=R, writes=R)
                        for g in range(4):
                            S.op("dve", lambda e, g=g: e.tensor_scalar(out=cmb[:, g * 4:(g + 1) * 4], in0=wi, scalar1=ohp[:, g:g + 1], scalar2=None,
                                                                       op0=ALU.mult), reads=R, writes=[B_cmb])
                        S.dma("sp", comb_d[c], cmb, reads=[B_cmb], writes=[B_cmbd[c]])
                        if dbg:
                            S.dma("sp", dbg_d["d_comb"][:, c, :], cmb, reads=[B_cmb])

                    chunks = list(range(cm0, cm0 + TSCM))
                    mldr = XLoader(x_d, chunks)
                    stageA(chunks[0])
                    for i, c in enumerate(chunks):
                        nxt = (lambda cc=chunks[i + 1]: stageA(cc)) if i + 1 < len(chunks) else None
                        tl = tails.pop(chunks[i - 1]) if i > 0 else None
                        run_multi([((lambda cc=c: stageB(cc)), 3), (nxt, 2), (tl, 1)])
                    tails.pop(chunks[-1])()
                    if stop == "m":
                        S.wait_all_dma("sp")
                    S.flush()
                    if stop == "m":
                        raise _Stop()

        with ExitStack() as bs:
            def lsb(name, shape, dt=F32):
                return sb(name, shape, dt, stack=bs)

            PH = [bs.enter_context(nc.psum_tensor("ph_%d" % i, [128, 1024], F32)) for i in range(2)]
            B_PH = [Buf("phk%d" % i, True) for i in range(2)]
            PO = [bs.enter_context(nc.psum_tensor("po_%d" % i, [128, 1024], F32)) for i in range(2)]
            B_PO = [Buf("pok%d" % i, True) for i in range(2)]
            w1r = Ring(bs, nc, "w1b", [128, 8, 512], BF16, 2)
            w3r = Ring(bs, nc, "w3b", [128, 8, 512], BF16, 2)
            w2r = Ring(bs, nc, "w2b", [128, 4, D], BF16, 2)
            h2T = lsb("h2T", [128, 8, TS], BF16)
            B_h2T = [Buf("h2T%d" % i) for i in range(TSC)]
            comb = lsb("comb", [128, TSC, 16]); B_comb = [Buf("comb%d" % i) for i in range(TSC)]
            acc = lsb("acc", [128, TSC, D]); B_acc = [Buf("acc%d" % i) for i in range(TSC)]
            bce = lsb("bce", [128, 2, D]); B_bce = Buf("bce")
            S.dma("sp", bce[:, 0, :], lnrows_d[4].partition_broadcast(128), writes=[B_bce])
            S.dma("sp", bce[:, 1, :], lnrows_d[5].partition_broadcast(128), writes=[B_bce])
            silr = Ring(bs, nc, "sil", [128, TT], F32, 2)
            actr = Ring(bs, nc, "actT", [128, 4, TT], BF16, 2)
            zr = Ring(bs, nc, "z2", [128, D], F32, 3)
            stt = Ring(bs, nc, "stt", [128, 16], F32, 3)
            phi = {"i": 0, "o": 0}
            wts = {}
            for st_i in range(NST):
                c0 = st_i * TSC
                def load_h2(cb):
                    for cl in range(TSC):
                        S.dma("sp", h2T[:, :, cl * 128:(cl + 1) * 128], h2T_d[cb + cl], reads=[B_h2d[cb + cl]], writes=[B_h2T[cl]])
                        S.dma("sp", comb[:, cl, :], comb_d[cb + cl], reads=[B_cmbd[cb + cl]], writes=[B_comb[cl]])
                if st_i == 0:
                    load_h2(c0)
                for cl in range(TSC):
                    S.dma("sp", acc[:, cl, :], x1a_d[(c0 + cl) * 128:(c0 + cl + 1) * 128, :], reads=[B_x1ad[c0 + cl]], writes=[B_acc[cl]])

                units = [(e_, t) for e_ in range(NE) for t in range(NT)]

                def load_w(e_):
                    B1, w1 = w1r.next(); B3, w3 = w3r.next(); B2, w2 = w2r.next()
                    S.dma("pool", w2, ew2_d[e_].rearrange("(k p) n -> p k n", p=128), writes=[B2])
                    S.dma("pool", w1, ew1_d[e_].rearrange("(k p) n -> p k n", p=128), writes=[B1])
                    S.dma("pool", w3, ew3_d[e_].rearrange("(k p) n -> p k n", p=128), writes=[B3])
                    for j in range(4):
                        S.op("pool", lambda e, j=j, w2=w2: e.tensor_tensor(out=w2[:, j, :], in0=w2[:, j, :], in1=g2bc[:], op=ALU.mult),
                             reads=[B_gbc], writes=[B2])
                    wts[e_] = (B1, w1, B3, w3, B2, w2)

                def phaseA(e_, t):
                    B1, w1, B3, w3, B2, w2 = wts[e_]
                    B_a, aT = actr.next()
                    rb = [B_h2T[t * NSUB + i] for i in range(NSUB)]
                    for j in range(4):
                        i = phi["i"]; phi["i"] = (i + 1) % 2
                        B_ph, ph = B_PH[i], PH[i][:]

                        def fn(e, j=j, ph=ph):
                            for k in range(8):
                                e.matmul(ph[:, 0:TT], lhsT=w1[:, k, j * 128:(j + 1) * 128], rhs=h2T[:, k, t * TT:(t + 1) * TT], start=(k == 0), stop=(k == 7))
                            for k in range(8):
                                ins = e.matmul(ph[:, 512:512 + TT], lhsT=w3[:, k, j * 128:(j + 1) * 128], rhs=h2T[:, k, t * TT:(t + 1) * TT],
                                               start=(k == 0), stop=(k == 7))
                            return ins
                        S.op("pe", fn, reads=rb + [B1, B3], writes=[B_ph])
                        B_s, sl = silr.next()
                        S.op("act", lambda e, sl=sl, ph=ph: e.activation(out=sl, in_=ph[:, 0:TT], func=AF.Silu), reads=[B_ph], writes=[B_s])
                        S.op("dve", lambda e, sl=sl, ph=ph, j=j: e.tensor_tensor(out=aT[:, j, :], in0=ph[:, 512:512 + TT], in1=sl, op=ALU.mult),
                             reads=[B_ph, B_s], writes=[B_a])
                    return B_a, aT

                def final_chunk(cl):
                    c = c0 + cl
                    B_z, z = zr.next()
                    B_st, st = stt.next()
                    for h in range(2):
                        S.op("dve", lambda e, h=h: e.bn_stats(out=st[:, h * 6:(h + 1) * 6], in_=acc[:, cl, h * 512:(h + 1) * 512]), reads=[B_acc[cl]], writes=[B_st])
                    S.op("dve", lambda e: e.bn_aggr(out=st[:, 12:14], in_=st[:, 0:12]), reads=[B_st], writes=[B_st])
                    S.op("act", lambda e: e.activation(out=st[:, 14:15], in_=st[:, 13:14], func=AF.Ln, bias=LN_EPS, scale=1.0), reads=[B_st], writes=[B_st])
                    S.op("act", lambda e: e.activation(out=st[:, 14:15], in_=st[:, 14:15], func=AF.Exp, scale=-0.5), reads=[B_st], writes=[B_st])
                    S.op("dve", lambda e: e.scalar_tensor_tensor(out=st[:, 15:16], in0=st[:, 12:13], scalar=-1.0, in1=st[:, 14:15],
                                                                 op0=ALU.mult, op1=ALU.mult), reads=[B_st], writes=[B_st])
                    S.op("act", lambda e: e.activation(out=z, in_=acc[:, cl, :], func=AF.Identity, bias=st[:, 15:16], scale=st[:, 14:15]),
                         reads=[B_st, B_acc[cl]], writes=[B_z])
                    eng2 = "dve" if cl % 2 == 0 else "pool"
                    S.op(eng2, lambda e: e.tensor_tensor(out=z, in0=z, in1=bce[:, 0, :], op=ALU.mult), reads=[B_bce], writes=[B_z])
                    S.op(eng2, lambda e: e.tensor_tensor(out=z, in0=z, in1=bce[:, 1, :], op=ALU.add), reads=[B_bce], writes=[B_z])
                    S.dma("sp", out_d[c * 128:(c + 1) * 128, :], z, reads=[B_z], writes=[B_out[c]])

                def phaseB(e_, t, B_a, aT):
                    B1, w1, B3, w3, B2, w2 = wts[e_]
                    for sub in range(NSUB):
                        cl = t * NSUB + sub
                        i = phi["o"]; phi["o"] = (i + 1) % 2
                        B_po, po = B_PO[i], PO[i][:]

                        def fn(e, sub=sub, po=po):
                            for hf in range(2):
                                for j in range(4):
                                    ins = e.matmul(po[:, hf * 512:(hf + 1) * 512], lhsT=aT[:, j, sub * 128:(sub + 1) * 128], rhs=w2[:, j, hf * 512:(hf + 1) * 512],
                                                   start=(j == 0), stop=(j == 3))
                            return ins
                        S.op("pe", fn, reads=[B_a, B2], writes=[B_po])
                        S.op("dve", lambda e, cl=cl, po=po: e.scalar_tensor_tensor(out=acc[:, cl, :], in0=po, scalar=comb[:, cl, e_:e_ + 1], in1=acc[:, cl, :],
                                                                                   op0=ALU.mult, op1=ALU.add),
                             reads=[B_po, B_comb[cl]], writes=[B_acc[cl]])

                if st_i == 0:
                    load_w(0)
                prev = None
                for ui, (e_, t) in enumerate(units):
                    B_a, aT = phaseA(e_, t)
                    if prev is not None:
                        phaseB(*prev)
                    prev = (e_, t, B_a, aT)
                    if t == 0 and e_ + 1 < NE:
                        load_w(e_ + 1)
                    elif t == 0 and e_ + 1 == NE and st_i + 1 < NST:
                        load_w(0)
                phaseB(*prev)
                if st_i + 1 < NST:
                    load_h2(c0 + TSC)
                run_pipeline([(lambda cl=cl: final_chunk(cl)) for cl in range(TSC)], 3, 4)
            S.wait_all_dma("sp")
            S.flush()


def _consts():
    s = np.arange(128)
    ident = np.eye(128, dtype=np.float32)
    v = np.float32(-1.0 / 16.0)
    texf = np.where(s[:, None] > s[None, :], v, np.float32(0)).astype(np.float32)
    texb = np.where(s[:, None] < s[None, :], v, np.float32(0)).astype(np.float32)
    tincf = np.where(s[:, None] <= s[None, :], v, np.float32(0)).astype(np.float32)
    tincb = np.where(s[:, None] >= s[None, :], v, np.float32(0)).astype(np.float32)
    m = np.where(s[:, None] < s[None, :], 1.0, 0.0) + 2.0 * np.eye(128)
    mfp = np.tile(m.astype(np.float32), (1, 4))
    mbs = np.tile((s[:, None] > s[None, :]).astype(np.int32), (1, 4))
    negcol = np.full((128, 2), v, dtype=np.float32)
    return dict(ident=ident, texf=texf, texb=texb, tincf=tincf, tincb=tincb, mfp=mfp, mbs=mbs, negcol=negcol)


def _colk(v):
    return np.ascontiguousarray(np.asarray(v, dtype=np.float32).reshape(8, 128).T)


def make_in_maps(inp, nb, SEQ):
    f = lambda a: np.ascontiguousarray(np.asarray(a, dtype=np.float32))
    b_ada = f(inp["b_ada"])[0]
    bada4 = np.concatenate([_colk(b_ada[i * D:(i + 1) * D]) for i in (0, 1, 3, 4)], axis=1)
    lncols = np.stack([_colk(inp["ln_in_g"]), _colk(inp["ln_in_b"]), _colk(f(inp["ln1_g"])[0]), _colk(f(inp["ln1_b"])[0])], axis=2)
    lnrows = np.stack([f(inp["ln_in_g"]), f(inp["ln_in_b"]), f(inp["ln1_g"])[0], f(inp["ln1_b"])[0], f(inp["ln2_g"])[0], f(inp["ln2_b"])[0]], axis=0)
    cw = f(inp["conv_w"])[0]
    cb = f(inp["conv_b"])[0]
    convcols = np.zeros((128, 4, 4), np.float32)
    for t in range(3):
        convcols[:, :, t] = cw[t].reshape(4, 128).T
    convcols[:, :, 3] = cb.reshape(4, 128).T
    gw_aug = np.zeros((33, 512), np.float32)
    gw_aug[0:16, 0:256] = f(inp["gate_w2_fwd"])[0]
    gw_aug[16:32, 256:512] = f(inp["gate_w2_bwd"])[0]
    gw_aug[32, 0:256] = f(inp["gate_b_fwd"])[0]
    gw_aug[32, 256:512] = f(inp["gate_b_bwd"])[0]
    wr = np.concatenate([f(inp["router_group_w"])[0], f(inp["router_expert_w"])[0]], axis=1)
    br = np.concatenate([f(inp["router_group_b"])[0], f(inp["router_expert_b"])[0]], axis=0)[None, :]
    shared = dict(
        w_ada=f(inp["w_ada"])[0], bada4=np.ascontiguousarray(bada4), bada_row=np.ascontiguousarray(b_ada[None, :]),
        lncols=np.ascontiguousarray(lncols), lnrows=np.ascontiguousarray(lnrows), w_in=f(inp["w_in"])[0],
        convcols=convcols, gw_aug=gw_aug, normg_col=np.ascontiguousarray(f(inp["gla_norm_g"])[0][:, None]),
        w_out=f(inp["w_out"])[0], wr=np.ascontiguousarray(wr), br_row=np.ascontiguousarray(br),
        ew1=f(inp["expert_w1"])[0], ew3=f(inp["expert_w3"])[0], ew2=f(inp["expert_w2"])[0],
    )
    shared.update(_consts())
    x = f(inp["x"]); c = f(inp["c"]); ctx = f(inp["ctx"]); c_ctx = f(inp["c_ctx"])
    maps = []
    for b in range(nb):
        m = dict(shared)
        m["x"] = np.ascontiguousarray(x[b, :SEQ])
        m["ctx"] = np.ascontiguousarray(ctx[b])
        m["ccol"] = np.ascontiguousarray(np.stack([_colk(c[b]), _colk(c_ctx)], axis=2))
        maps.append(m)
    return maps


_CACHE = {}


def kernel(**inputs):
    x = np.asarray(inputs["x"])
    nb, SEQ = x.shape[0], x.shape[1]
    key = (SEQ,)
    if key not in _CACHE:
        _CACHE[key] = build_program(SEQ, 8 if SEQ >= 1024 else SEQ // 128)
    nc = _CACHE[key]
    maps = make_in_maps(inputs, nb, SEQ)
    res = run_bass_kernel_spmd(nc, maps, core_ids=list(range(nb)))
    out = np.stack([np.asarray(r["out"], dtype=np.float32) for r in res.results], axis=0)
    return out
```

```python
import math
import threading
from contextlib import ExitStack

import numpy as np
import concourse.bass as bass
import concourse.mybir as mybir
from concourse.bass_utils import run_bass_kernel_spmd

F32 = mybir.dt.float32
BF16 = mybir.dt.bfloat16
I32 = mybir.dt.int32
AF = mybir.ActivationFunctionType
ALU = mybir.AluOpType
AX = mybir.AxisListType

D = 1024
DPROJ = 3104
NE = 16
ALPHA = 2.0 ** 0.25
LN_EPS = 1e-5
RMS_EPS = 1e-6


class Buf:
    __slots__ = ("name", "w", "r", "dead", "psum")

    def __init__(self, name, psum=False):
        self.name = name
        self.w = None
        self.r = {}
        self.dead = False
        self.psum = psum

    def regen(self):
        n = Buf(self.name, self.psum)
        n.w = self.w
        n.r = dict(self.r)
        self.dead = True
        return n


class Sched:
    NAMES = ("pe", "act", "dve", "pool", "sp")
    ATTR = {"pe": "tensor", "act": "scalar", "dve": "vector", "pool": "gpsimd", "sp": "sync"}

    def __init__(self, nc, es, nds=32):
        self.nc = nc
        self.sem = {e: es.enter_context(nc.semaphore("s_" + e)) for e in self.NAMES}
        self.cnt = {e: 0 for e in self.NAMES}
        self.dsem = [es.enter_context(nc.semaphore("sd%d" % i)) for i in range(nds)]
        self.dval = [0] * nds
        self.dnext = 0
        self.dnext2 = 0
        self.ops = {e: [] for e in self.NAMES}
        self.waited = {e: {} for e in self.NAMES}

    def semof(self, k):
        return self.sem[k] if isinstance(k, str) else self.dsem[k]

    def _deps(self, eng, reads, writes):
        w = {}

        def add(ev):
            if ev is None:
                return
            k, v = ev
            if k == eng:
                if eng == "pe":
                    return
            if w.get(k, 0) < v:
                w[k] = v

        for b in reads:
            assert not b.dead, "use of re-allocated buffer %s" % b.name
            add(b.w)
            if b.psum:
                for k, v in b.r.items():
                    if k != eng:
                        add((k, v))
        for b in writes:
            assert not b.dead, "use of re-allocated buffer %s" % b.name
            add(b.w)
            for k, v in b.r.items():
                add((k, v))
        out = []
        wd = self.waited[eng]
        for k, v in w.items():
            if wd.get(k, 0) >= v:
                continue
            wd[k] = v
            out.append((k, v))
        return out

    @staticmethod
    def _mark(ev, reads, writes):
        k, v = ev
        for b in reads:
            if b.r.get(k, 0) < v:
                b.r[k] = v
        for b in writes:
            b.w = ev
            b.r = {}

    def op(self, eng, fn, reads=(), writes=()):
        waits = self._deps(eng, reads, writes)
        self.cnt[eng] += 1
        ev = (eng, self.cnt[eng])
        self._mark(ev, reads, writes)
        self.ops[eng].append((waits, fn, ("inc", eng)))
        if Coro.cur is not None:
            Coro.cur.yield_()

    def dma(self, q, out_ap, in_ap, reads=(), writes=()):
        half = len(self.dsem) // 2
        if q == "sp":
            i = self.dnext
            self.dnext = (self.dnext + 1) % half
        else:
            i = half + self.dnext2
            self.dnext2 = (self.dnext2 + 1) % half
        waits = self._deps(q, reads, writes)
        prev = self.dval[i]
        if prev > 0 and self.waited[q].get(i, 0) < prev:
            self.waited[q][i] = prev
            waits.append((i, prev))
        self.dval[i] += 16
        ev = (i, self.dval[i])
        self._mark(ev, reads, writes)
        self.ops[q].append((waits, (lambda e, o=out_ap, s=in_ap: e.dma_start(out=o, in_=s)), ("dma", i)))
        if Coro.cur is not None:
            Coro.cur.yield_()

    def wait_all_dma(self, q):
        waits = []
        for i, v in enumerate(self.dval):
            if v > 0 and self.waited[q].get(i, 0) < v:
                self.waited[q][i] = v
                waits.append((i, v))
        self.ops[q].append((waits, None, None))

    def flush(self):
        nc = self.nc
        self.wait_all_dma("sp")
        with nc.Block() as blk:
            for e in self.NAMES:
                ops = self.ops[e]
                if not ops:
                    continue

                def body(eng, ops=ops):
                    for waits, fn, tag in ops:
                        for k, v in waits:
                            eng.wait_ge(self.semof(k), v)
                        if fn is None:
                            continue
                        ins = fn(eng)
                        if tag[0] == "inc":
                            ins.then_inc(self.sem[tag[1]], 1)
                        else:
                            ins.then_inc(self.dsem[tag[1]], 16)

                getattr(blk, self.ATTR[e])(body)
        self.ops = {e: [] for e in self.NAMES}


class _Stop(Exception):
    pass


class Coro:
    cur = None

    def __init__(self, fn):
        self.fn = fn
        self.done = False
        self.nops = 0
        self.exc = None
        self.go = threading.Semaphore(0)
        self.back = threading.Semaphore(0)
        self.t = threading.Thread(target=self._run, daemon=True)
        self.started = False

    def _run(self):
        self.go.acquire()
        try:
            self.fn()
        except BaseException as e:
            self.exc = e
        self.done = True
        self.back.release()

    def step(self):
        if self.done:
            return
        if not self.started:
            self.started = True
            self.t.start()
        prev = Coro.cur
        Coro.cur = self
        self.go.release()
        self.back.acquire()
        Coro.cur = prev
        if self.exc is not None:
            raise self.exc

    def yield_(self):
        self.nops += 1
        self.back.release()
        self.go.acquire()


def run_pipeline(bodies, max_active, lag):
    active = []
    i = 0
    while i < len(bodies) or active:
        if i < len(bodies) and len(active) < max_active and (not active or active[-1].nops >= lag):
            active.append(Coro(bodies[i]))
            i += 1
        for co in list(active):
            co.step()
            if co.done:
                active.remove(co)


def run_multi(items):
    cos = [(Coro(f), q) for f, q in items if f is not None]
    while any(not c.done for c, _ in cos):
        for c, q in cos:
            for _ in range(q):
                c.step()


def run_pair(fb, fa, qb=2, qa=1):
    cb = Coro(fb)
    ca = Coro(fa) if fa is not None else None
    while not cb.done or (ca is not None and not ca.done):
        for _ in range(qb):
            cb.step()
        if ca is not None:
            for _ in range(qa):
                ca.step()


class Ring:
    UID = 0

    def __init__(self, es, nc, name, shape, dtype, n):
        self.n = n
        Ring.UID += 1
        self.t = es.enter_context(nc.sbuf_tensor("%s_r%d" % (name, Ring.UID), [shape[0], n] + list(shape[1:]), dtype))
        self.bufs = [Buf("%s%d" % (name, i)) for i in range(n)]
        self.i = 0

    def next(self):
        i = self.i
        self.i = (self.i + 1) % self.n
        self.bufs[i] = self.bufs[i].regen()
        return self.bufs[i], self.t[:, i]


def build_program(SEQ, TSC, CTX=256, dbg=False, stop=None):
    NCH = SEQ // 128
    NST = NCH // TSC
    TS = TSC * 128
    TT = min(512, TS)
    NT = TS // TT
    NSUB = TT // 128
    NCC = CTX // 128

    nc = bass.Bass("TRN2", target_bir_lowering=False)

    def din(name, shape, dt=F32):
        return nc.dram_tensor(name, list(shape), dt, kind="ExternalInput").ap()

    x_d = din("x", [SEQ, D])
    ctx_d = din("ctx", [CTX, D])
    ccol_d = din("ccol", [128, 8, 2])
    wada_d = din("w_ada", [D, 6 * D])
    bada4_d = din("bada4", [128, 32])
    badarow_d = din("bada_row", [1, 6 * D])
    lncols_d = din("lncols", [128, 8, 4])
    lnrows_d = din("lnrows", [6, D])
    win_d = din("w_in", [D, DPROJ])
    convc_d = din("convcols", [128, 4, 4])
    gwaug_d = din("gw_aug", [33, 512])
    normg_d = din("normg_col", [128, 1])
    wout_d = din("w_out", [D, D])
    wr_d = din("wr", [D, 20])
    br_d = din("br_row", [1, 20])
    ew1_d = din("ew1", [NE, D, 512])
    ew3_d = din("ew3", [NE, D, 512])
    ew2_d = din("ew2", [NE, 512, D])
    ident_d = din("ident", [128, 128])
    texf_d = din("texf", [128, 128])
    texb_d = din("texb", [128, 128])
    tincf_d = din("tincf", [128, 128])
    tincb_d = din("tincb", [128, 128])
    mfp_d = din("mfp", [128, 512])
    mbs_d = din("mbs", [128, 512], I32)
    negcol_d = din("negcol", [128, 2])
    out_d = nc.dram_tensor("out", [SEQ, D], F32, kind="ExternalOutput").ap()
    x1a_d = nc.dram_tensor("x1a_scr", [SEQ, D], F32, kind="Internal").ap()
    sbscr_d = nc.dram_tensor("sb_scr", [NCH, 128, 2, 128], BF16, kind="Internal").ap()
    h2T_d = nc.dram_tensor("h2t_scr", [NCH, 128, 8, 128], BF16, kind="Internal").ap()
    comb_d = nc.dram_tensor("comb_scr", [NCH, 128, 16], F32, kind="Internal").ap()
    dbg_d = {}
    if dbg:
        for nm, shp in (("d_x1a", [SEQ, D]), ("d_sf", [128, 512]), ("d_sb", [128, 512]),
                        ("d_comb", [128, NCH, 16]), ("d_h2t", [128, 8, TS])):
            dbg_d[nm] = nc.dram_tensor(nm, shp, F32, kind="ExternalOutput").ap()

    es_all = ExitStack()
    try:
        _build_body(nc, es_all, locals())
    except _Stop:
        pass
    return nc


NPIPE, LAGP = 4, 12


def _build_body(nc, es_all, L):
    globals_ = L
    (SEQ, TSC, CTX, dbg, stop, NCH, NST, TS, TT, NT, NSUB, NCC) = (L[k] for k in ("SEQ", "TSC", "CTX", "dbg", "stop", "NCH", "NST", "TS", "TT", "NT", "NSUB", "NCC"))
    (x_d, ctx_d, ccol_d, wada_d, bada4_d, badarow_d, lncols_d, lnrows_d, win_d, convc_d, gwaug_d, normg_d, wout_d, wr_d, br_d,
     ew1_d, ew3_d, ew2_d, ident_d, texf_d, texb_d, tincf_d, tincb_d, mfp_d, mbs_d, negcol_d, out_d, x1a_d, dbg_d, sbscr_d, h2T_d, comb_d) = (
        L[k] for k in ("x_d", "ctx_d", "ccol_d", "wada_d", "bada4_d", "badarow_d", "lncols_d", "lnrows_d", "win_d", "convc_d", "gwaug_d",
                       "normg_d", "wout_d", "wr_d", "br_d", "ew1_d", "ew3_d", "ew2_d", "ident_d", "texf_d", "texb_d", "tincf_d", "tincb_d",
                       "mfp_d", "mbs_d", "negcol_d", "out_d", "x1a_d", "dbg_d", "sbscr_d", "h2T_d", "comb_d"))
    with es_all as es:
        S = Sched(nc, es)

        def chk(name):
            if stop == name:
                S.wait_all_dma("sp")
                S.flush()
                raise _Stop()

        uid = {"n": 0}

        def sb(name, shape, dt=F32, stack=es):
            uid["n"] += 1
            return stack.enter_context(nc.sbuf_tensor("%s_s%d" % (name, uid["n"]), list(shape), dt))

        ident = sb("ident", [128, 128]); B_ident = Buf("ident")
        identb = sb("identb", [128, 128], BF16)
        texf = sb("texf", [128, 128]); texb = sb("texb", [128, 128])
        tincf = sb("tincf", [128, 128]); tincb = sb("tincb", [128, 128])
        negcol = sb("negcol", [128, 2])
        B_const = Buf("consts")
        lncols = sb("lncols", [128, 8, 4])
        convc = sb("convc", [128, 4, 4])
        gwaug = sb("gwaug", [33, 512])
        normg = sb("normg", [128, 1])
        cols = sb("cols", [128, 6, 8])
        B_cols = Buf("cols")
        g2bc = sb("g2bc", [128, D]); B_gbc = Buf("gbc")
        wrp = sb("wrp", [128, 8, 20]); brp = sb("brp", [128, 20]); B_wrp = Buf("wrp")
        Sf = sb("Sf", [128, 512]); B_Sf = Buf("Sf")
        B_Sb = Buf("Sb")
        Sfb = Ring(es, nc, "Sfb", [128, 512], BF16, 2)
        B_sbpl = [Buf("sbpl%d" % c) for c in range(NCH)]
        B_h2d = [Buf("h2d%d" % c) for c in range(NCH)]
        B_cmbd = [Buf("cmbd%d" % c) for c in range(NCH)]
        B_x1ad = [Buf("x1ad%d" % c) for c in range(NCH)]
        win = sb("win", [128, 8, DPROJ], BF16)
        B_winl = [Buf("win%d" % i) for i in range(16)]
        wout = sb("wout", [128, 8, D], BF16); B_wout = Buf("wout")
        B_out = [Buf("out%d" % c) for c in range(NCH)]

        def ld_const(t, src, bufs):
            S.dma("sp", t[:], src, writes=bufs)

        with ExitStack() as bs:
            def lsb(name, shape, dt=F32):
                return sb(name, shape, dt, stack=bs)

            def ps(name):
                return bs.enter_context(nc.psum_tensor(name, [128, 1024], F32))

            PB = [ps("pb%d" % i) for i in range(4)]
            B_PB = [Buf("pbk%d" % i, True) for i in range(8)]
            pstate = {"i": 0}

            def bank():
                i = pstate["i"]
                pstate["i"] = (i + 1) % 8
                B_PB[i] = B_PB[i].regen()
                return B_PB[i], PB[i // 2][:, (i % 2) * 512:(i % 2) * 512 + 512]

            def bank2():
                if pstate["i"] % 2:
                    pstate["i"] = (pstate["i"] + 1) % 8
                i = pstate["i"]
                pstate["i"] = (i + 2) % 8
                B_PB[i] = B_PB[i].regen()
                B_PB[i + 1] = B_PB[i + 1].regen()
                return [B_PB[i], B_PB[i + 1]], PB[i // 2][:]

            ones = lsb("ones", [128, 128])
            g1bc = lsb("g1bc", [128, D])
            Sb = lsb("Sb", [128, 512])
            for t, src in ((ident, ident_d), (texf, texf_d), (texb, texb_d), (tincf, tincf_d),
                           (tincb, tincb_d), (negcol, negcol_d),
                           (lncols, lncols_d), (convc, convc_d), (gwaug, gwaug_d), (normg, normg_d)):
                ld_const(t, src, [B_const])
            for k in range(8):
                S.dma("pool", win[:, k, 0:1552], win_d[k * 128:(k + 1) * 128, 0:1552], writes=[B_winl[2 * k]])
                S.dma("pool", win[:, k, 1552:DPROJ], win_d[k * 128:(k + 1) * 128, 1552:DPROJ], writes=[B_winl[2 * k + 1]])
            S.dma("pool", wout[:], wout_d.rearrange("(k p) n -> p k n", p=128), writes=[B_wout])
            S.op("dve", lambda e: e.memset(ones[:], 1.0), writes=[B_const])
            S.op("dve", lambda e: e.tensor_copy(out=identb[:], in_=ident[:]), reads=[B_const], writes=[B_ident])
            S.op("dve", lambda e: e.memset(Sf[:], 0.0), writes=[B_Sf])
            S.op("dve", lambda e: e.memset(Sb[:], 0.0), writes=[B_Sb])

            ccol = lsb("ccol", [128, 8, 2]); B_cc = Buf("ccol")
            scs = lsb("scs", [128, 8, 2])
            scb = lsb("scb", [128, 8, 128]); B_scb = Buf("scb")
            bada4 = lsb("bada4", [128, 32])
            modc = lsb("modc", [128, 32, 2]); B_mod = Buf("mod")
            S.dma("sp", ccol[:], ccol_d, writes=[B_cc])
            S.dma("sp", bada4[:], bada4_d, writes=[B_cc])
            S.dma("sp", g1bc[:], badarow_d[0, 2 * D:3 * D].partition_broadcast(128), writes=[B_gbc])
            S.dma("sp", g2bc[:], badarow_d[0, 5 * D:6 * D].partition_broadcast(128), writes=[B_gbc])
            S.op("act", lambda e: e.activation(out=scs[:], in_=ccol[:], func=AF.Silu), reads=[B_cc], writes=[B_cc])
            for k in range(8):
                S.op("dve", lambda e, k=k: e.tensor_scalar(out=scb[:, k, :], in0=ones[:], scalar1=scs[:, k, 0:1],
                                                           scalar2=None, op0=ALU.mult),
                     reads=[B_cc, B_const], writes=[B_scb])
            slab = Ring(bs, nc, "slab", [128, 8, 512], F32, 2)
            B_pcol, pcol = bank()
            colkind = {0: 0, 1: 1, 3: 2, 4: 3}
            for s in range(12):
                kind, half = s // 2, s % 2
                B_sl, sl = slab.next()
                S.dma("sp", sl, wada_d[:, s * 512:(s + 1) * 512].rearrange("(k p) n -> p k n", p=128), writes=[B_sl])
                if kind in colkind:
                    for f in range(4):
                        idx = colkind[kind] * 8 + half * 4 + f

                        def fn(e, sl=sl, f=f, idx=idx):
                            for k in range(8):
                                ins = e.matmul(pcol[:, idx * 2:idx * 2 + 2], lhsT=sl[:, k, f * 128:(f + 1) * 128],
                                               rhs=scs[:, k, :], start=(k == 0), stop=(k == 7))
                            return ins
                        S.op("pe", fn, reads=[B_sl, B_cc], writes=[B_pcol])
                else:
                    B_pg, pg = bank()

                    def fn(e, sl=sl, pg=pg):
                        for k in range(8):
                            ins = e.matmul(pg, lhsT=scb[:, k, :], rhs=sl[:, k, :], start=(k == 0), stop=(k == 7))
                        return ins
                    S.op("pe", fn, reads=[B_sl, B_scb], writes=[B_pg])
                    gt = g1bc if kind == 2 else g2bc
                    S.op("dve", lambda e, gt=gt, pg=pg, half=half: e.tensor_tensor(
                        out=gt[:, half * 512:(half + 1) * 512], in0=pg, in1=gt[:, half * 512:(half + 1) * 512], op=ALU.add),
                        reads=[B_pg, B_gbc], writes=[B_gbc])
            for j in range(2):
                S.op("dve", lambda e, j=j: e.tensor_tensor(out=modc[:, :, j], in0=pcol[:, 0:64].rearrange("p (i t) -> p i t", t=2)[:, :, j], in1=bada4[:], op=ALU.add),
                     reads=[B_pcol, B_cc], writes=[B_mod])
            tmpc = lsb("tmpc", [128, 8])

            def derive(gi, bi, g_idx, b_idx, j, sh_off, sc_off):
                S.op("dve", lambda e: e.tensor_scalar(out=tmpc[:], in0=modc[:, sc_off:sc_off + 8, j], scalar1=1.0,
                                                      scalar2=None, op0=ALU.add), reads=[B_mod], writes=[B_mod])
                S.op("dve", lambda e: e.tensor_tensor(out=cols[:, gi, :], in0=lncols[:, :, g_idx], in1=tmpc[:], op=ALU.mult),
                     reads=[B_mod, B_const], writes=[B_cols])
                S.op("dve", lambda e: e.tensor_tensor(out=tmpc[:], in0=lncols[:, :, b_idx], in1=tmpc[:], op=ALU.mult),
                     reads=[B_mod, B_const], writes=[B_mod])
                S.op("dve", lambda e: e.tensor_tensor(out=cols[:, bi, :], in0=tmpc[:], in1=modc[:, sh_off:sh_off + 8, j], op=ALU.add),
                     reads=[B_mod], writes=[B_cols])

            derive(0, 1, 0, 1, 0, 0, 8)
            derive(2, 3, 0, 1, 1, 0, 8)
            derive(4, 5, 2, 3, 0, 16, 24)
            wrs = lsb("wrs", [128, 8, 20]); B_wrs = Buf("wrs")
            S.dma("sp", wrs[:], wr_d.rearrange("(k p) n -> p k n", p=128), writes=[B_wrs])
            S.dma("sp", brp[:], br_d[0].partition_broadcast(128), writes=[B_wrp])
            for k in range(8):
                S.op("dve", lambda e, k=k: e.tensor_scalar(out=wrp[:, k, :], in0=wrs[:, k, :], scalar1=cols[:, 4, k:k + 1],
                                                           scalar2=None, op0=ALU.mult),
                     reads=[B_wrs, B_cols], writes=[B_wrp])
                S.op("dve", lambda e, k=k: e.tensor_scalar(out=scb[:, k, :], in0=ones[:], scalar1=cols[:, 5, k:k + 1],
                                                           scalar2=None, op0=ALU.mult),
                     reads=[B_cols, B_const], writes=[B_scb])
            B_pb2, pb2 = bank()

            def fn(e):
                for k in range(8):
                    ins = e.matmul(pb2[:, 0:20], lhsT=scb[:, k, :], rhs=wrs[:, k, :], start=(k == 0), stop=(k == 7))
                return ins
            S.op("pe", fn, reads=[B_scb, B_wrs], writes=[B_pb2])
            S.op("dve", lambda e: e.tensor_tensor(out=brp[:], in0=pb2[:, 0:20], in1=brp[:], op=ALU.add),
                 reads=[B_pb2, B_wrp], writes=[B_wrp])

            for k in range(8):
                if k < 4:
                    S.op("dve", lambda e, k=k: e.tensor_tensor(out=wout[:, k, :], in0=wout[:, k, :], in1=g1bc[:], op=ALU.mult),
                         reads=[B_gbc, B_wout], writes=[B_wout])
                else:
                    S.op("dve", lambda e, k=k: e.scalar_tensor_tensor(out=wout[:, k, :], in0=wout[:, k, :], scalar=normg[:, 0:1], in1=g1bc[:],
                                                                      op0=ALU.mult, op1=ALU.mult),
                         reads=[B_gbc, B_wout, B_const], writes=[B_wout])
            if stop == "p0a":
                chk("p0a")

            xin = Ring(bs, nc, "xin", [128, D], F32, 4)
            xhat = Ring(bs, nc, "xhat", [128, D], F32, 4)
            hTr = Ring(bs, nc, "hT", [128, 8, 128], BF16, 4)
            stt = Ring(bs, nc, "stt", [128, 16], F32, 4)
            lowaug = Ring(bs, nc, "lowaug", [33, 128], F32, 4)
            e1r = Ring(bs, nc, "e1", [128, 512], F32, 4)
            Gr = Ring(bs, nc, "G", [128, 512], F32, 4)
            ekr = Ring(bs, nc, "ek", [128, 256], F32, 4)
            kendr = Ring(bs, nc, "kend", [128, 256], BF16, 4)
            vbr = Ring(bs, nc, "vb", [128, 512], BF16, 4)
            decr = Ring(bs, nc, "dec", [128, 4], F32, 4)
            sbstr = Ring(bs, nc, "sbst", [128, 2, 128], BF16, 4)
            for i in range(4):
                S.op("dve", lambda e, i=i: e.memset(lowaug.t[:, i], 1.0), writes=[lowaug.bufs[i]])

            class XLoader:
                def __init__(self, src_d, order):
                    self.src_d, self.order, self.q, self.nxt = src_d, list(order), {}, 0

                def get(self, c):
                    i = self.order.index(c)
                    depth = xin.n - 1
                    while self.nxt < len(self.order) and self.nxt <= i + depth:
                        cc = self.order[self.nxt]
                        B_x, xt = xin.next()
                        S.dma("sp", xt, self.src_d[cc * 128:(cc + 1) * 128, :], writes=[B_x])
                        self.q[cc] = (B_x, xt)
                        self.nxt += 1
                    return self.q.pop(c)

            def front(ldr, c, gi, bi, xh_ring=xhat, keep=None):
                B_x, xt = ldr.get(c)
                B_st, st = stt.next()
                for h in range(2):
                    S.op("dve", lambda e, h=h, st=st, xt=xt: e.bn_stats(out=st[:, h * 6:(h + 1) * 6], in_=xt[:, h * 512:(h + 1) * 512]),
                         reads=[B_x], writes=[B_st])
                S.op("dve", lambda e, st=st: e.bn_aggr(out=st[:, 12:14], in_=st[:, 0:12]), reads=[B_st], writes=[B_st])
                S.op("act", lambda e, st=st: e.activation(out=st[:, 14:15], in_=st[:, 13:14], func=AF.Ln, bias=LN_EPS, scale=1.0),
                     reads=[B_st], writes=[B_st])
                S.op("act", lambda e, st=st: e.activation(out=st[:, 14:15], in_=st[:, 14:15], func=AF.Exp, scale=-0.5),
                     reads=[B_st], writes=[B_st])
                S.op("dve", lambda e, st=st: e.scalar_tensor_tensor(out=st[:, 15:16], in0=st[:, 12:13], scalar=-1.0, in1=st[:, 14:15],
                                                                    op0=ALU.mult, op1=ALU.mult), reads=[B_st], writes=[B_st])
                B_xh, xh = xh_ring.next()
                S.op("act", lambda e, st=st, xt=xt, xh=xh: e.activation(out=xh, in_=xt, func=AF.Identity, bias=st[:, 15:16], scale=st[:, 14:15]),
                     reads=[B_st, B_x], writes=[B_xh])
                B_tp, tp = bank2()

                def fn(e, xh=xh, tp=tp):
                    for k in range(8):
                        ins = e.transpose(out=tp[:, k * 128:(k + 1) * 128], in_=xh[:, k * 128:(k + 1) * 128], identity=ident[:])
                    return ins
                S.op("pe", fn, reads=[B_xh, B_const], writes=B_tp)
                B_h, hT = hTr.next()
                for kk in range(8):
                    k = (kk // 2) + (4 if kk % 2 else 0)
                    if k < 4:
                        S.op("act", lambda e, k=k, hT=hT, tp=tp: e.activation(out=hT[:, k, :], in_=tp[:, k * 128:(k + 1) * 128], func=AF.Identity,
                                                                             bias=cols[:, bi, k:k + 1], scale=cols[:, gi, k:k + 1]),
                             reads=[B_tp[0], B_cols], writes=[B_h])
                    else:
                        S.op("dve", lambda e, k=k, hT=hT, tp=tp: e.tensor_scalar(out=hT[:, k, :], in0=tp[:, k * 128:(k + 1) * 128],
                                                                                scalar1=cols[:, gi, k:k + 1], scalar2=cols[:, bi, k:k + 1],
                                                                                op0=ALU.mult, op1=ALU.add),
                             reads=[B_tp[1], B_cols], writes=[B_h])
                return B_xh, xh, B_h, hT, B_tp, tp

            def gates(B_h, hT, wt, B_wt, goff):
                B_pl, pl = bank()

                def fn(e):
                    for k in range(8):
                        ins = e.matmul(pl[0:32, 0:128], lhsT=wt[:, k, goff:goff + 32], rhs=hT[:, k, :], start=(k == 0), stop=(k == 7))
                    return ins
                S.op("pe", fn, reads=[B_h] + B_wt, writes=[B_pl])
                B_la, la = lowaug.next()
                S.op("act", lambda e: e.activation(out=la[0:32, :], in_=pl[0:32, 0:128], func=AF.Copy), reads=[B_pl], writes=[B_la])
                B_pz, pz = bank()
                S.op("pe", lambda e: e.matmul(pz, lhsT=la[0:33, :], rhs=gwaug[:], start=True, stop=True),
                     reads=[B_la, B_const], writes=[B_pz])
                B_e1, e1 = e1r.next()
                S.op("act", lambda e: e.activation(out=e1, in_=pz, func=AF.Exp, scale=-1.0), reads=[B_pz], writes=[B_e1])
                B_G, G = Gr.next()
                S.op("act", lambda e: e.activation(out=G, in_=e1, func=AF.Ln, bias=1.0, scale=1.0), reads=[B_e1], writes=[B_G])
                return B_G, G

            def kend_and_decay(B_G, G, d, pk, B_pk):
                tex = texf if d == 0 else texb
                B_pe, pe_ = bank()
                S.op("pe", lambda e: e.matmul(pe_[:, 0:256], lhsT=tex[:], rhs=G[:, d * 256:(d + 1) * 256], start=True, stop=True),
                     reads=[B_G, B_const], writes=[B_pe])
                B_ek, ek = ekr.next()
                S.op("act", lambda e: e.activation(out=ek, in_=pe_[:, 0:256], func=AF.Exp), reads=[B_pe], writes=[B_ek])
                B_ke, ke = kendr.next()
                S.op("dve", lambda e: e.tensor_tensor(out=ke, in0=pk, in1=ek, op=ALU.mult), reads=[B_pk, B_ek], writes=[B_ke])
                B_pd, pd = bank()

                def fn(e):
                    for pt in range(2):
                        ins = e.matmul(pd[:, pt * 2:pt * 2 + 2], lhsT=G[:, d * 256 + pt * 128:d * 256 + (pt + 1) * 128], rhs=negcol[:],
                                       start=True, stop=True)
                    return ins
                S.op("pe", fn, reads=[B_G, B_const], writes=[B_pd])
                B_dc, dc = decr.next()
                S.op("act", lambda e: e.activation(out=dc, in_=pd[:, 0:4], func=AF.Exp), reads=[B_pd], writes=[B_dc])
                return B_ke, ke, B_dc, dc

            def state_update(St, B_St, B_ke, ke, B_vb, vb, B_dc, dc):
                B_pu, pu = bank()

                def fn(e):
                    for pt in range(2):
                        ins = e.matmul(pu[:, pt * 256:(pt + 1) * 256], lhsT=ke[:, pt * 128:(pt + 1) * 128], rhs=vb[:, pt * 256:(pt + 1) * 256],
                                       start=True, stop=True)
                    return ins
                S.op("pe", fn, reads=[B_ke, B_vb], writes=[B_pu])
                for pt in range(2):
                    for hf in range(2):
                        rs = slice(hf * 64, (hf + 1) * 64)
                        cs = slice(pt * 256 + hf * 128, pt * 256 + (hf + 1) * 128)
                        S.op("dve", lambda e, rs=rs, cs=cs, pt=pt: e.scalar_tensor_tensor(
                            out=St[rs, cs], in0=St[rs, cs], scalar=dc[rs, pt * 2:pt * 2 + 1], in1=pu[rs, cs], op0=ALU.mult, op1=ALU.add),
                            reads=[B_pu, B_dc, B_St], writes=[B_St])

            def state_pass(src_d, order, d, gi, bi, St, B_St, store):
                ldr = XLoader(src_d, order)
                run_pipeline([(lambda c=c: state_chunk(ldr, c, d, gi, bi, St, B_St, store)) for c in order], NPIPE, LAGP)

            def state_chunk(ldr, c, d, gi, bi, St, B_St, store):
                B_xh, xh, B_h, hT, _, _ = front(ldr, c, gi, bi)
                B_G, G = gates(B_h, hT, win, B_winl, 3072)
                tex = texf if d == 0 else texb
                B_pe, pe_ = bank()
                S.op("pe", lambda e: e.matmul(pe_[:, 0:256], lhsT=tex[:], rhs=G[:, d * 256:(d + 1) * 256], start=True, stop=True),
                     reads=[B_G, B_const], writes=[B_pe])
                B_ek, ek = ekr.next()
                S.op("act", lambda e: e.activation(out=ek, in_=pe_[:, 0:256], func=AF.Exp), reads=[B_pe], writes=[B_ek])
                B_pd, pd = bank()

                def fn(e):
                    for pt in range(2):
                        ins = e.matmul(pd[:, pt * 2:pt * 2 + 2], lhsT=G[:, d * 256 + pt * 128:d * 256 + (pt + 1) * 128], rhs=negcol[:],
                                       start=True, stop=True)
                    return ins
                S.op("pe", fn, reads=[B_G, B_const], writes=[B_pd])
                B_dc, dc = decr.next()
                S.op("act", lambda e: e.activation(out=dc, in_=pd[:, 0:4], func=AF.Exp), reads=[B_pd], writes=[B_dc])
                B_pk, pk = bank()

                def fn(e):
                    for k in range(8):
                        ins = e.matmul(pk[:, 0:256], lhsT=hT[:, k, :], rhs=win[:, k, 1792:2048], start=(k == 0), stop=(k == 7))
                    return ins
                S.op("pe", fn, reads=[B_h] + B_winl, writes=[B_pk])
                B_ke, ke = kendr.next()
                S.op("dve", lambda e: e.tensor_tensor(out=ke, in0=pk[:, 0:256], in1=ek, op=ALU.mult), reads=[B_pk, B_ek], writes=[B_ke])
                B_pv, pv = bank()

                def fn(e):
                    for k in range(8):
                        ins = e.matmul(pv, lhsT=hT[:, k, :], rhs=win[:, k, 2048:2560], start=(k == 0), stop=(k == 7))
                    return ins
                S.op("pe", fn, reads=[B_h] + B_winl, writes=[B_pv])
                B_vb, vb = vbr.next()
                S.op("act", lambda e: e.activation(out=vb, in_=pv, func=AF.Copy), reads=[B_pv], writes=[B_vb])
                if store:
                    B_sbt, sbt = sbstr.next()
                    for pt in range(2):
                        for hf in range(2):
                            rs = slice(hf * 64, (hf + 1) * 64)
                            cs = slice(pt * 256 + hf * 128, pt * 256 + (hf + 1) * 128)
                            S.op("pool", lambda e, rs=rs, cs=cs, pt=pt: e.tensor_copy(out=sbt[rs, pt, :], in_=St[rs, cs]),
                                 reads=[B_St], writes=[B_sbt])
                    S.dma("sp", sbscr_d[c], sbt, reads=[B_sbt], writes=[B_sbpl[c]])
                state_update(St, B_St, B_ke, ke, B_vb, vb, B_dc, dc)

            state_pass(ctx_d, list(range(NCC)), 0, 2, 3, Sf, B_Sf, False)
            state_pass(ctx_d, list(range(NCC - 1, -1, -1)), 1, 2, 3, Sb, B_Sb, False)
            state_pass(x_d, list(range(NCH - 1, -1, -1)), 1, 0, 1, Sb, B_Sb, True)
            B_sf0, sf0 = Sfb.next()
            S.op("act", lambda e: e.activation(out=sf0, in_=Sf[:], func=AF.Copy), reads=[B_Sf], writes=[B_sf0])
            if dbg:
                S.dma("sp", dbg_d["d_sf"], Sf[:], reads=[B_Sf])
                S.dma("sp", dbg_d["d_sb"], Sb[:], reads=[B_Sb])
            if stop == "p0":
                S.wait_all_dma("sp")
            S.flush()
            if stop == "p0":
                raise _Stop()
        cur_sfb = {"B": B_sf0, "ap": sf0}

        for st_i in range(1):
            c0 = 0
            if st_i == 0:
                cm0, TSCM = 0, NCH
                with ExitStack() as bs:
                    def lsb(name, shape, dt=F32):
                        return sb(name, shape, dt, stack=bs)

                    PB = [bs.enter_context(nc.psum_tensor("pm%d_%d" % (st_i, i), [128, 1024], F32)) for i in range(4)]
                    B_PB = [Buf("pmk%d" % i, True) for i in range(8)]
                    pstate = {"A": 0, "B": 0}

                    def _pool():
                        return getattr(threading.current_thread(), "pool", "A")

                    def bank():
                        p = _pool()
                        j = pstate[p]
                        pstate[p] = (j + 1) % 4
                        i = j + (4 if p == "B" else 0)
                        B_PB[i] = B_PB[i].regen()
                        return B_PB[i], PB[i // 2][:, (i % 2) * 512:(i % 2) * 512 + 512]

                    def bank2():
                        p = _pool()
                        if pstate[p] % 2:
                            pstate[p] = (pstate[p] + 1) % 4
                        j = pstate[p]
                        pstate[p] = (j + 2) % 4
                        i = j + (4 if p == "B" else 0)
                        B_PB[i] = B_PB[i].regen()
                        B_PB[i + 1] = B_PB[i + 1].regen()
                        return [B_PB[i], B_PB[i + 1]], PB[i // 2][:]

                    sbpl = lsb("sbpl", [128, TSCM, 2, 128], BF16)
                    B_sbl = [Buf("sbl%d" % i) for i in range(TSCM)]
                    for i in range(TSCM):
                        S.dma("sp", sbpl[:, i], sbscr_d[cm0 + i], reads=[B_sbpl[cm0 + i]], writes=[B_sbl[i]])
                    mfp = lsb("mfp", [128, 512]); mbs = lsb("mbs", [128, 512], I32); B_msk = Buf("masks")
                    S.dma("sp", mfp[:], mfp_d, writes=[B_msk])
                    S.dma("sp", mbs[:], mbs_d, writes=[B_msk])
                    bct = lsb("bct", [128, 4, D]); B_bct = Buf("bct")
                    for i, r in enumerate((0, 1, 2, 3)):
                        S.dma("sp", bct[:, i, :], lnrows_d[r].partition_broadcast(128), writes=[B_bct])
                    S.op("act", lambda e: e.activation(out=bct[:], in_=bct[:], func=AF.Copy, scale=ALPHA), reads=[B_bct], writes=[B_bct])

                    xin = Ring(bs, nc, "xin", [128, D], F32, 2)
                    xhat = Ring(bs, nc, "xhat", [128, D], F32, 2)
                    hTr = Ring(bs, nc, "hT", [128, 8, 128], BF16, 2)
                    stt = Ring(bs, nc, "stt", [128, 16], F32, 4)
                    lowaug = Ring(bs, nc, "lowaug", [33, 128], F32, 2)
                    e1r = Ring(bs, nc, "e1", [128, 512], F32, 1)
                    Gr = Ring(bs, nc, "G", [128, 512], F32, 2)
                    ekr = Ring(bs, nc, "ek", [128, 256], F32, 1)
                    kendr = Ring(bs, nc, "kend", [128, 256], BF16, 2)
                    vbr = Ring(bs, nc, "vb", [128, 512], BF16, 2)
                    decr = Ring(bs, nc, "dec", [128, 4], F32, 2)
                    axr = Ring(bs, nc, "ax", [128, 512], F32, 1)
                    ur = Ring(bs, nc, "u", [128, 512], F32, 1)
                    cvr = Ring(bs, nc, "cv", [128, 512], F32, 1)
                    yar = Ring(bs, nc, "yaT", [128, 4, 128], BF16, 2)
                    qkr = Ring(bs, nc, "qk", [128, 4, 128], F32, 2)
                    ktr = Ring(bs, nc, "ktm", [128, 256], F32, 2)
                    srr = Ring(bs, nc, "sr", [128, 512], F32, 2)
                    eqr = Ring(bs, nc, "eq", [128, 4, 128], F32, 1)
                    ekk = Ring(bs, nc, "ekk", [128, 4, 128], F32, 1)
                    qdr = Ring(bs, nc, "qd", [128, 4, 128], BF16, 1)
                    kir = Ring(bs, nc, "ki", [128, 4, 128], BF16, 1)
                    qbd = lsb("qbd", [128, 4, 256], BF16); B_qbd = Buf("qbd")
                    sbd = lsb("sbd", [128, 512], BF16); B_sbd = Buf("sbd")
                    atr = Ring(bs, nc, "AT", [128, 512], BF16, 1)
                    ssr = Ring(bs, nc, "ss", [128, 16], F32, 2)
                    junk = lsb("junk", [128, 128]); B_junk = Buf("junk")
                    ybr = Ring(bs, nc, "yb", [128, 512], BF16, 1)
                    ybTr = Ring(bs, nc, "ybT", [128, 4, 128], BF16, 1)
                    x1hr = Ring(bs, nc, "x1h", [128, D], F32, 1)
                    x1Tr = Ring(bs, nc, "x1T", [128, D], F32, 1)
                    rtr = Ring(bs, nc, "rt", [128, 64], F32, 2)
                    h2r = Ring(bs, nc, "h2t", [128, 8, 128], BF16, 2)
                    cmbr = Ring(bs, nc, "cmb", [128, 16], F32, 3)
                    for i in range(2):
                        S.op("dve", lambda e, i=i: e.memset(lowaug.t[:, i], 1.0), writes=[lowaug.bufs[i]])
                    S.op("dve", lambda e: e.memset(qbd[:], 0.0), writes=[B_qbd])
                    S.op("dve", lambda e: e.memset(sbd[:], 0.0), writes=[B_sbd])

                    stash = {}
                    tails = {}

                    def stageA(c):
                        threading.current_thread().pool = "A"
                        cl = c - cm0
                        B_xh, xh, B_h, hT, _, _ = front(mldr, c, 0, 1, xh_ring=xhat)
                        B_G, G = gates(B_h, hT, win, B_winl, 3072)
                        B_pq, pq = bank()

                        def fn(e, hT=hT, pq=pq):
                            for g in range(4):
                                for k in range(8):
                                    ins = e.matmul(pq[:, g * 128:(g + 1) * 128], lhsT=win[:, k, 1536 + g * 128:1536 + (g + 1) * 128], rhs=hT[:, k, :],
                                                   start=(k == 0), stop=(k == 7))
                            return ins
                        S.op("pe", fn, reads=[B_h] + B_winl, writes=[B_pq])
                        B_qk, qk = qkr.next()
                        S.op("act", lambda e, qk=qk, pq=pq: e.activation(out=qk.rearrange("p a b -> p (a b)"), in_=pq, func=AF.Copy), reads=[B_pq], writes=[B_qk])
                        B_pk, pk = bank()
                        B_pv, pv = bank()
                        B_pr, pr = bank()

                        def fn(e, hT=hT, pk=pk, pv=pv, pr=pr):
                            for k in range(8):
                                e.matmul(pk[:, 0:256], lhsT=hT[:, k, :], rhs=win[:, k, 1792:2048], start=(k == 0), stop=(k == 7))
                            for k in range(8):
                                e.matmul(pv, lhsT=hT[:, k, :], rhs=win[:, k, 2048:2560], start=(k == 0), stop=(k == 7))
                            for k in range(8):
                                ins = e.matmul(pr, lhsT=hT[:, k, :], rhs=win[:, k, 2560:3072], start=(k == 0), stop=(k == 7))
                            return ins
                        S.op("pe", fn, reads=[B_h] + B_winl, writes=[B_pk, B_pv, B_pr])
                        B_kt, kt = ktr.next()
                        S.op("act", lambda e, kt=kt, pk=pk: e.activation(out=kt, in_=pk[:, 0:256], func=AF.Copy), reads=[B_pk], writes=[B_kt])
                        B_vb, vb = vbr.next()
                        S.op("act", lambda e, vb=vb, pv=pv: e.activation(out=vb, in_=pv, func=AF.Copy), reads=[B_pv], writes=[B_vb])
                        B_sr, sr = srr.next()
                        S.op("act", lambda e, sr=sr, pr=pr: e.activation(out=sr, in_=pr, func=AF.Silu), reads=[B_pr], writes=[B_sr])
                        B_ya, ya = yar.next()
                        B_pcc, pcc = bank()
                        B_pcx, pcx = bank()
                        B_pcb, pcb = bank()

                        def fn(e, hT=hT, pcc=pcc, pcx=pcx, pcb=pcb):
                            for off, pbk in ((512, pcc), (1024, pcx), (0, pcb)):
                                for j in range(4):
                                    for k in range(8):
                                        ins = e.matmul(pbk[:, j * 128:(j + 1) * 128], lhsT=win[:, k, off + j * 128:off + (j + 1) * 128], rhs=hT[:, k, :],
                                                       start=(k == 0), stop=(k == 7))
                            return ins
                        S.op("pe", fn, reads=[B_h] + B_winl, writes=[B_pcc, B_pcx, B_pcb])
                        B_ax, ax = axr.next()
                        S.op("act", lambda e, ax=ax, pcx=pcx: e.activation(out=ax, in_=pcx, func=AF.Copy), reads=[B_pcx], writes=[B_ax])
                        B_u, u = ur.next()
                        S.op("dve", lambda e, u=u, ax=ax, pcc=pcc: e.tensor_tensor(out=u, in0=pcc, in1=ax, op=ALU.mult),
                             reads=[B_pcc, B_ax], writes=[B_u])
                        B_cv, cv = cvr.next()
                        for j in range(4):
                            S.op("act", lambda e, cv=cv, u=u, j=j: e.activation(out=cv[:, j * 128:(j + 1) * 128], in_=u[:, j * 128:(j + 1) * 128], func=AF.Identity,
                                                                             bias=convc[:, j, 3:4], scale=convc[:, j, 1:2]),
                                 reads=[B_u, B_const], writes=[B_cv])
                        for j in range(4):
                            u3 = u[:, j * 128:(j + 1) * 128].rearrange("p (r w) -> p r w", w=64)
                            cv3 = cv[:, j * 128:(j + 1) * 128].rearrange("p (r w) -> p r w", w=64)
                            S.op("dve", lambda e, u3=u3, cv3=cv3, j=j: e.scalar_tensor_tensor(out=cv3[:, :, 1:64], in0=u3[:, :, 0:63], scalar=convc[:, j, 0:1],
                                                                                             in1=cv3[:, :, 1:64], op0=ALU.mult, op1=ALU.add),
                                 reads=[B_u, B_cv, B_const], writes=[B_cv])
                            S.op("dve", lambda e, u3=u3, cv3=cv3, j=j: e.scalar_tensor_tensor(out=cv3[:, :, 0:63], in0=u3[:, :, 1:64], scalar=convc[:, j, 2:3],
                                                                                             in1=cv3[:, :, 0:63], op0=ALU.mult, op1=ALU.add),
                                 reads=[B_u, B_cv, B_const], writes=[B_cv])
                        S.op("dve", lambda e, ya=ya, cv=cv, pcb=pcb: e.tensor_tensor(out=ya.rearrange("p a b -> p (a b)"), in0=pcb, in1=cv, op=ALU.mult),
                             reads=[B_pcb, B_cv], writes=[B_ya])
                        chk("a1")
                        chk("a2")
                        S.op("pool", lambda e, xh=xh: e.tensor_tensor(out=xh, in0=xh, in1=bct[:, 0, :], op=ALU.mult), reads=[B_bct], writes=[B_xh])
                        S.op("pool", lambda e, xh=xh: e.tensor_tensor(out=xh, in0=xh, in1=bct[:, 1, :], op=ALU.add), reads=[B_bct], writes=[B_xh])
                        chk("a3")
                        stash[c] = dict(B_xh=B_xh, xh=xh, B_ya=B_ya, ya=ya, B_qk=B_qk, qk=qk, B_kt=B_kt, kt=kt, B_vb=B_vb, vb=vb,
                                        B_sr=B_sr, sr=sr, B_G=B_G, G=G)

                    def stageB(c):
                        threading.current_thread().pool = "B"
                        cl = c - cm0
                        s = stash.pop(c)
                        B_G, G, qk, B_qk, vb, B_vb = s["B_G"], s["G"], s["qk"], s["B_qk"], s["vb"], s["B_vb"]
                        for pt in range(2):
                            for hf in range(2):
                                rs = slice(hf * 64, (hf + 1) * 64)
                                cs = slice(pt * 256 + hf * 128, pt * 256 + (hf + 1) * 128)
                                S.op("pool", lambda e, rs=rs, cs=cs, pt=pt: e.tensor_copy(out=sbd[rs, cs], in_=sbpl[rs, cl, pt, :]),
                                     reads=[B_sbl[cl]], writes=[B_sbd])
                        B_pg, pg = bank()

                        def fn(e):
                            for d in range(2):
                                tinc = tincf if d == 0 else tincb
                                for pt in range(2):
                                    i = d * 2 + pt
                                    ins = e.matmul(pg[:, i * 128:(i + 1) * 128], lhsT=G[:, d * 256 + pt * 128:d * 256 + (pt + 1) * 128], rhs=tinc[:],
                                                   start=True, stop=True)
                            return ins
                        S.op("pe", fn, reads=[B_G, B_const], writes=[B_pg])
                        B_eq, eq = eqr.next()
                        B_ek2, ek2 = ekk.next()
                        S.op("act", lambda e: e.activation(out=eq.rearrange("p a b -> p (a b)"), in_=pg, func=AF.Exp, bias=math.log(0.125), scale=1.0),
                             reads=[B_pg], writes=[B_eq])
                        S.op("act", lambda e: e.activation(out=ek2.rearrange("p a b -> p (a b)"), in_=pg, func=AF.Exp, scale=-1.0), reads=[B_pg], writes=[B_ek2])
                        B_qd, qd = qdr.next()
                        B_ki, ki = kir.next()
                        for d in range(2):
                            S.op("dve", lambda e, d=d: e.tensor_tensor(out=qd[:, d * 2:d * 2 + 2, :], in0=qk[:, 0:2, :], in1=eq[:, d * 2:d * 2 + 2, :], op=ALU.mult),
                                 reads=[B_qk, B_eq], writes=[B_qd])
                            S.op("dve", lambda e, d=d: e.tensor_tensor(out=ki[:, d * 2:d * 2 + 2, :], in0=qk[:, 2:4, :], in1=ek2[:, d * 2:d * 2 + 2, :], op=ALU.mult),
                                 reads=[B_qk, B_ek2], writes=[B_ki])
                        for hf in range(2):
                            rs = slice(hf * 64, (hf + 1) * 64)
                            S.op("act", lambda e, rs=rs, hf=hf: e.activation(out=qbd[rs, :, hf * 128:(hf + 1) * 128], in_=qd[rs, :, :], func=AF.Copy),
                                 reads=[B_qd], writes=[B_qbd])
                        chk("b1")
                        pAs = []
                        for d in range(2):
                            B_pa, pa = bank()

                            def fn(e, d=d, pa=pa):
                                for pt in range(2):
                                    i = d * 2 + pt
                                    ins = e.matmul(pa[:, pt * 256:(pt + 1) * 256], lhsT=ki[:, i, :], rhs=qbd[:, i, :], start=True, stop=True)
                                return ins
                            S.op("pe", fn, reads=[B_ki, B_qbd], writes=[B_pa])
                            pAs.append((B_pa, pa))
                        B_at, at = atr.next()
                        S.op("dve", lambda e: e.tensor_tensor(out=at, in0=pAs[0][1], in1=mfp[:], op=ALU.mult), reads=[pAs[0][0], B_msk], writes=[B_at])
                        S.op("dve", lambda e: e.copy_predicated(out=at, mask=mbs[:], data=pAs[1][1]), reads=[pAs[1][0], B_msk, B_at], writes=[B_at])
                        chk("b2")
                        B_sfc, sfc = cur_sfb["B"], cur_sfb["ap"]
                        B_po, po = bank()

                        def fn(e):
                            for h in range(4):
                                e.matmul(po[:, h * 128:(h + 1) * 128], lhsT=at[:, h * 128:(h + 1) * 128], rhs=vb[:, h * 128:(h + 1) * 128],
                                         start=(h == 0), stop=False, skip_group_check=True)
                            for pt in range(2):
                                e.matmul(po[:, pt * 256:(pt + 1) * 256], lhsT=qd[:, pt, :], rhs=sfc[:, pt * 256:(pt + 1) * 256],
                                         start=False, stop=False, skip_group_check=True)
                            for pt in range(2):
                                ins = e.matmul(po[:, pt * 256:(pt + 1) * 256], lhsT=qd[:, 2 + pt, :], rhs=sbd[:, pt * 256:(pt + 1) * 256],
                                               start=False, stop=(pt == 1), skip_group_check=True)
                            return ins
                        S.op("pe", fn, reads=[B_at, B_vb, B_qd, B_sfc, B_sbd], writes=[B_po])
                        chk("b3")
                        B_ke, ke, B_dc, dc = kend_and_decay(B_G, G, 0, s["kt"], s["B_kt"])
                        state_update(Sf, B_Sf, B_ke, ke, B_vb, vb, B_dc, dc)
                        B_sfn, sfn = Sfb.next()
                        S.op("act", lambda e: e.activation(out=sfn, in_=Sf[:], func=AF.Copy), reads=[B_Sf], writes=[B_sfn])
                        cur_sfb["B"], cur_sfb["ap"] = B_sfn, sfn
                        chk("b4")
                        B_ss, ss = ssr.next()
                        for h in range(4):
                            S.op("act", lambda e, h=h: e.activation(out=junk[:], in_=po[:, h * 128:(h + 1) * 128], func=AF.Square, accum_out=ss[:, h:h + 1]),
                                 reads=[B_po], writes=[B_junk, B_ss])
                        S.op("act", lambda e: e.activation(out=ss[:, 4:8], in_=ss[:, 0:4], func=AF.Ln, bias=RMS_EPS, scale=1.0 / 128.0), reads=[B_ss], writes=[B_ss])
                        S.op("act", lambda e: e.activation(out=ss[:, 8:12], in_=ss[:, 4:8], func=AF.Exp, scale=-0.5), reads=[B_ss], writes=[B_ss])
                        B_yb, yb = ybr.next()
                        sr = s["sr"]
                        for h in range(4):
                            S.op("dve", lambda e, h=h: e.scalar_tensor_tensor(out=yb[:, h * 128:(h + 1) * 128], in0=po[:, h * 128:(h + 1) * 128],
                                                                              scalar=ss[:, 8 + h:9 + h], in1=sr[:, h * 128:(h + 1) * 128],
                                                                              op0=ALU.mult, op1=ALU.mult),
                                 reads=[B_po, B_ss, s["B_sr"]], writes=[B_yb])
                        B_pt, pt_ = bank()
                        ptb = pt_.bitcast(BF16)

                        def fn(e):
                            for h in range(4):
                                ins = e.transpose(out=ptb[:, h * 128:(h + 1) * 128], in_=yb[:, h * 128:(h + 1) * 128], identity=identb[:])
                            return ins
                        S.op("pe", fn, reads=[B_yb, B_ident], writes=[B_pt])
                        B_ybT, ybT = ybTr.next()
                        S.op("act", lambda e: e.activation(out=ybT.rearrange("p a b -> p (a b)"), in_=ptb[:, 0:512], func=AF.Copy), reads=[B_pt], writes=[B_ybT])
                        chk("b5")
                        B_py, py = bank2()
                        ya = s["ya"]

                        def fn(e):
                            for hf in range(2):
                                for k in range(8):
                                    lhsT = ya[:, k, :] if k < 4 else ybT[:, k - 4, :]
                                    ins = e.matmul(py[:, hf * 512:(hf + 1) * 512], lhsT=lhsT, rhs=wout[:, k, hf * 512:(hf + 1) * 512],
                                                   start=(k == 0), stop=(k == 7))
                            return ins
                        S.op("pe", fn, reads=[s["B_ya"], B_ybT, B_wout], writes=B_py)
                        B_z1, z1 = s["B_xh"], s["xh"]
                        S.op("dve", lambda e: e.tensor_tensor(out=z1, in0=py, in1=s["xh"], op=ALU.add), reads=B_py + [s["B_xh"]], writes=[B_z1])
                        B_st, st = stt.next()
                        for h in range(2):
                            S.op("dve", lambda e, h=h: e.bn_stats(out=st[:, h * 6:(h + 1) * 6], in_=z1[:, h * 512:(h + 1) * 512]), reads=[B_z1], writes=[B_st])
                        S.op("dve", lambda e: e.bn_aggr(out=st[:, 12:14], in_=st[:, 0:12]), reads=[B_st], writes=[B_st])
                        S.op("act", lambda e: e.activation(out=st[:, 14:15], in_=st[:, 13:14], func=AF.Ln, bias=LN_EPS, scale=1.0), reads=[B_st], writes=[B_st])
                        S.op("act", lambda e: e.activation(out=st[:, 14:15], in_=st[:, 14:15], func=AF.Exp, scale=-0.5), reads=[B_st], writes=[B_st])
                        S.op("dve", lambda e: e.scalar_tensor_tensor(out=st[:, 15:16], in0=st[:, 12:13], scalar=-1.0, in1=st[:, 14:15],
                                                                     op0=ALU.mult, op1=ALU.mult), reads=[B_st], writes=[B_st])
                        B_x1h, x1h = x1hr.next()
                        S.op("act", lambda e: e.activation(out=x1h, in_=z1, func=AF.Identity, bias=st[:, 15:16], scale=st[:, 14:15]),
                             reads=[B_st, B_z1], writes=[B_x1h])
                        chk("b6")
                        B_tp, tp = bank2()

                        def fn(e):
                            for k in range(8):
                                ins = e.transpose(out=tp[:, k * 128:(k + 1) * 128], in_=x1h[:, k * 128:(k + 1) * 128], identity=ident[:])
                            return ins
                        S.op("pe", fn, reads=[B_x1h, B_const], writes=B_tp)
                        S.op("pool", lambda e: e.tensor_tensor(out=x1h, in0=x1h, in1=bct[:, 2, :], op=ALU.mult), reads=[B_bct], writes=[B_x1h])
                        S.op("pool", lambda e: e.tensor_tensor(out=x1h, in0=x1h, in1=bct[:, 3, :], op=ALU.add), reads=[B_bct], writes=[B_x1h])
                        chk("b6a")
                        S.dma("sp", x1a_d[c * 128:(c + 1) * 128, :], x1h, reads=[B_x1h], writes=[B_x1ad[c]])
                        chk("b6b")
                        if dbg:
                            S.dma("sp", dbg_d["d_x1a"][c * 128:(c + 1) * 128, :], x1h, reads=[B_x1h])
                        chk("b6d")
                        B_x1T, x1T = x1Tr.next()
                        S.op("act", lambda e: e.activation(out=x1T[:, 0:512], in_=tp[:, 0:512], func=AF.Copy), reads=[B_tp[0]], writes=[B_x1T])
                        S.op("dve", lambda e: e.tensor_copy(out=x1T[:, 512:1024], in_=tp[:, 512:1024]), reads=[B_tp[1]], writes=[B_x1T])
                        chk("b6c")
                        B_prt, prt = bank()

                        def fn(e):
                            for k in range(8):
                                ins = e.matmul(prt[:, 0:20], lhsT=x1T[:, k * 128:(k + 1) * 128], rhs=wrp[:, k, :], start=(k == 0), stop=(k == 7))
                            return ins
                        S.op("pe", fn, reads=[B_x1T, B_wrp], writes=[B_prt])
                        chk("b7")
                        B_rt, rt = rtr.next()
                        S.op("dve", lambda e: e.tensor_tensor(out=rt[:, 0:20], in0=prt[:, 0:20], in1=brp[:], op=ALU.add), reads=[B_prt, B_wrp], writes=[B_rt])
                        B_h2t, h2t = h2r.next()
                        for kk in range(8):
                            k = (kk // 2) + (4 if kk % 2 else 0)
                            if k < 4:
                                S.op("act", lambda e, k=k: e.activation(out=h2t[:, k, :], in_=tp[:, k * 128:(k + 1) * 128], func=AF.Identity,
                                                                        bias=cols[:, 5, k:k + 1], scale=cols[:, 4, k:k + 1]),
                                     reads=[B_tp[0], B_cols], writes=[B_h2t])
                            else:
                                S.op("dve", lambda e, k=k: e.tensor_scalar(out=h2t[:, k, :], in0=tp[:, k * 128:(k + 1) * 128],
                                                                           scalar1=cols[:, 4, k:k + 1], scalar2=cols[:, 5, k:k + 1],
                                                                           op0=ALU.mult, op1=ALU.add),
                                     reads=[B_tp[1], B_cols], writes=[B_h2t])
                        S.dma("sp", h2T_d[c], h2t, reads=[B_h2t], writes=[B_h2d[c]])
                        tails[c] = lambda: route_tail(c, cl, B_rt, rt)

                    def route_tail(c, cl, B_rt, rt):
                        B_cmb, cmb = cmbr.next()
                        R = [B_rt]
                        lg = rt[:, 0:20]
                        mg = rt[:, 20:21]; eg = rt[:, 21:25]; sg = rt[:, 25:26]; pgp = rt[:, 26:27]; oh = rt[:, 27:31]
                        sel = rt[:, 31:35]; m2 = rt[:, 35:36]; es_ = rt[:, 36:40]; mk1 = rt[:, 40:44]; esm = rt[:, 44:48]
                        e2 = rt[:, 48:49]; mk2 = rt[:, 49:53]; den = rt[:, 53:54]; wi = rt[:, 54:58]; ohp = rt[:, 58:62]; nm = rt[:, 62:64]
                        S.op("dve", lambda e: e.tensor_reduce(out=mg, in_=rt[:, 0:4], axis=AX.X, op=ALU.max), reads=R, writes=R)
                        S.op("dve", lambda e: e.tensor_scalar(out=nm[:, 0:1], in0=mg, scalar1=-1.0, scalar2=None, op0=ALU.mult), reads=R, writes=R)
                        S.op("act", lambda e: e.activation(out=eg, in_=rt[:, 0:4], func=AF.Exp, bias=nm[:, 0:1], scale=1.0, accum_out=sg), reads=R, writes=R)
                        S.op("dve", lambda e: e.reciprocal(out=pgp, in_=sg), reads=R, writes=R)
                        S.op("dve", lambda e: e.tensor_scalar(out=oh, in0=rt[:, 0:4], scalar1=mg, scalar2=None, op0=ALU.is_equal), reads=R, writes=R)
                        S.op("dve", lambda e: e.tensor_scalar(out=sel, in0=rt[:, 4:8], scalar1=oh[:, 0:1], scalar2=None, op0=ALU.mult), reads=R, writes=R)
                        for g in range(1, 4):
                            S.op("dve", lambda e, g=g: e.scalar_tensor_tensor(out=sel, in0=rt[:, 4 + 4 * g:8 + 4 * g], scalar=oh[:, g:g + 1], in1=sel,
                                                                              op0=ALU.mult, op1=ALU.add), reads=R, writes=R)
                        S.op("dve", lambda e: e.tensor_reduce(out=m2, in_=sel, axis=AX.X, op=ALU.max), reads=R, writes=R)
                        S.op("dve", lambda e: e.tensor_scalar(out=nm[:, 1:2], in0=m2, scalar1=-1.0, scalar2=None, op0=ALU.mult), reads=R, writes=R)
                        S.op("act", lambda e: e.activation(out=es_, in_=sel, func=AF.Exp, bias=nm[:, 1:2], scale=1.0), reads=R, writes=R)
                        S.op("dve", lambda e: e.tensor_scalar(out=mk1, in0=sel, scalar1=m2, scalar2=None, op0=ALU.is_equal), reads=R, writes=R)
                        S.op("dve", lambda e: e.scalar_tensor_tensor(out=esm, in0=mk1, scalar=-2.0, in1=es_, op0=ALU.mult, op1=ALU.add), reads=R, writes=R)
                        S.op("dve", lambda e: e.tensor_reduce(out=e2, in_=esm, axis=AX.X, op=ALU.max), reads=R, writes=R)
                        S.op("dve", lambda e: e.tensor_scalar(out=mk2, in0=esm, scalar1=e2, scalar2=None, op0=ALU.is_equal), reads=R, writes=R)
                        S.op("dve", lambda e: e.tensor_scalar(out=den, in0=e2, scalar1=1.0, scalar2=None, op0=ALU.add), reads=R, writes=R)
                        S.op("dve", lambda e: e.reciprocal(out=den, in_=den), reads=R, writes=R)
                        S.op("dve", lambda e: e.tensor_tensor(out=wi, in0=mk1, in1=mk2, op=ALU.add), reads=R, writes=R)
                        S.op("dve", lambda e: e.tensor_tensor(out=wi, in0=wi, in1=es_, op=ALU.mult), reads=R, writes=R)
                        S.op("dve", lambda e: e.tensor_scalar(out=wi, in0=wi, scalar1=den, scalar2=None, op0=ALU.mult), reads=R, writes=R)
                        S.op("dve", lambda e: e.tensor_scalar(out=ohp, in0=oh, scalar1=pgp, scalar2=None, op0=ALU.mult), reads=R, writes=R)
                        for g in range(4):
                            S.op("dve", lambda e, g=g: e.tensor_scalar(out=cmb[:, g * 4:(g + 1) * 4], in0=wi, scalar1=ohp[:, g:g + 1], scalar2=None,
                                                                       op0=ALU.mult), reads=R, writes=[B_cmb])
                        S.dma("sp", comb_d[c], cmb, reads=[B_cmb], writes=[B_cmbd[c]])
                        if dbg:
                            S.dma("sp", dbg_d["d_comb"][:, c, :], cmb, reads=[B_cmb])

                    chunks = list(range(cm0, cm0 + TSCM))
                    mldr = XLoader(x_d, chunks)
                    stageA(chunks[0])
                    for i, c in enumerate(chunks):
                        nxt = (lambda cc=chunks[i + 1]: stageA(cc)) if i + 1 < len(chunks) else None
                        tl = tails.pop(chunks[i - 1]) if i > 0 else None
                        run_multi([((lambda cc=c: stageB(cc)), 3), (nxt, 2), (tl, 1)])
                    tails.pop(chunks[-1])()
                    if stop == "m":
                        S.wait_all_dma("sp")
                    S.flush()
                    if stop == "m":
                        raise _Stop()

        with ExitStack() as bs:
            def lsb(name, shape, dt=F32):
                return sb(name, shape, dt, stack=bs)

            PH = [bs.enter_context(nc.psum_tensor("ph_%d" % i, [128, 1024], F32)) for i in range(2)]
            B_PH = [Buf("phk%d" % i, True) for i in range(2)]
            PO = [bs.enter_context(nc.psum_tensor("po_%d" % i, [128, 1024], F32)) for i in range(2)]
            B_PO = [Buf("pok%d" % i, True) for i in range(2)]
            w1r = Ring(bs, nc, "w1b", [128, 8, 512], BF16, 2)
            w3r = Ring(bs, nc, "w3b", [128, 8, 512], BF16, 2)
            w2r = Ring(bs, nc, "w2b", [128, 4, D], BF16, 2)
            h2T = lsb("h2T", [128, 8, TS], BF16)
            B_h2T = [Buf("h2T%d" % i) for i in range(TSC)]
            comb = lsb("comb", [128, TSC, 16]); B_comb = [Buf("comb%d" % i) for i in range(TSC)]
            acc = lsb("acc", [128, TSC, D]); B_acc = [Buf("acc%d" % i) for i in range(TSC)]
            bce = lsb("bce", [128, 2, D]); B_bce = Buf("bce")
            S.dma("sp", bce[:, 0, :], lnrows_d[4].partition_broadcast(128), writes=[B_bce])
            S.dma("sp", bce[:, 1, :], lnrows_d[5].partition_broadcast(128), writes=[B_bce])
            silr = Ring(bs, nc, "sil", [128, TT], F32, 2)
            actr = Ring(bs, nc, "actT", [128, 4, TT], BF16, 2)
            zr = Ring(bs, nc, "z2", [128, D], F32, 3)
            stt = Ring(bs, nc, "stt", [128, 16], F32, 3)
            phi = {"i": 0, "o": 0}
            wts = {}
            for st_i in range(NST):
                c0 = st_i * TSC
                def load_h2(cb):
                    for cl in range(TSC):
                        S.dma("sp", h2T[:, :, cl * 128:(cl + 1) * 128], h2T_d[cb + cl], reads=[B_h2d[cb + cl]], writes=[B_h2T[cl]])
                        S.dma("sp", comb[:, cl, :], comb_d[cb + cl], reads=[B_cmbd[cb + cl]], writes=[B_comb[cl]])
                if st_i == 0:
                    load_h2(c0)
                for cl in range(TSC):
                    S.dma("sp", acc[:, cl, :], x1a_d[(c0 + cl) * 128:(c0 + cl + 1) * 128, :], reads=[B_x1ad[c0 + cl]], writes=[B_acc[cl]])

                units = [(e_, t) for e_ in range(NE) for t in range(NT)]

                def load_w(e_):
                    B1, w1 = w1r.next(); B3, w3 = w3r.next(); B2, w2 = w2r.next()
                    S.dma("pool", w2, ew2_d[e_].rearrange("(k p) n -> p k n", p=128), writes=[B2])
                    S.dma("pool", w1, ew1_d[e_].rearrange("(k p) n -> p k n", p=128), writes=[B1])
                    S.dma("pool", w3, ew3_d[e_].rearrange("(k p) n -> p k n", p=128), writes=[B3])
                    for j in range(4):
                        S.op("pool", lambda e, j=j, w2=w2: e.tensor_tensor(out=w2[:, j, :], in0=w2[:, j, :], in1=g2bc[:], op=ALU.mult),
                             reads=[B_gbc], writes=[B2])
                    wts[e_] = (B1, w1, B3, w3, B2, w2)

                def phaseA(e_, t):
                    B1, w1, B3, w3, B2, w2 = wts[e_]
                    B_a, aT = actr.next()
                    rb = [B_h2T[t * NSUB + i] for i in range(NSUB)]
                    for j in range(4):
                        i = phi["i"]; phi["i"] = (i + 1) % 2
                        B_ph, ph = B_PH[i], PH[i][:]

                        def fn(e, j=j, ph=ph):
                            for k in range(8):
                                e.matmul(ph[:, 0:TT], lhsT=w1[:, k, j * 128:(j + 1) * 128], rhs=h2T[:, k, t * TT:(t + 1) * TT], start=(k == 0), stop=(k == 7))
                            for k in range(8):
                                ins = e.matmul(ph[:, 512:512 + TT], lhsT=w3[:, k, j * 128:(j + 1) * 128], rhs=h2T[:, k, t * TT:(t + 1) * TT],
                                               start=(k == 0), stop=(k == 7))
                            return ins
                        S.op("pe", fn, reads=rb + [B1, B3], writes=[B_ph])
                        B_s, sl = silr.next()
                        S.op("act", lambda e, sl=sl, ph=ph: e.activation(out=sl, in_=ph[:, 0:TT], func=AF.Silu), reads=[B_ph], writes=[B_s])
                        S.op("dve", lambda e, sl=sl, ph=ph, j=j: e.tensor_tensor(out=aT[:, j, :], in0=ph[:, 512:512 + TT], in1=sl, op=ALU.mult),
                             reads=[B_ph, B_s], writes=[B_a])
                    return B_a, aT

                def final_chunk(cl):
                    c = c0 + cl
                    B_z, z = zr.next()
                    B_st, st = stt.next()
                    for h in range(2):
                        S.op("dve", lambda e, h=h: e.bn_stats(out=st[:, h * 6:(h + 1) * 6], in_=acc[:, cl, h * 512:(h + 1) * 512]), reads=[B_acc[cl]], writes=[B_st])
                    S.op("dve", lambda e: e.bn_aggr(out=st[:, 12:14], in_=st[:, 0:12]), reads=[B_st], writes=[B_st])
                    S.op("act", lambda e: e.activation(out=st[:, 14:15], in_=st[:, 13:14], func=AF.Ln, bias=LN_EPS, scale=1.0), reads=[B_st], writes=[B_st])
                    S.op("act", lambda e: e.activation(out=st[:, 14:15], in_=st[:, 14:15], func=AF.Exp, scale=-0.5), reads=[B_st], writes=[B_st])
                    S.op("dve", lambda e: e.scalar_tensor_tensor(out=st[:, 15:16], in0=st[:, 12:13], scalar=-1.0, in1=st[:, 14:15],
                                                                 op0=ALU.mult, op1=ALU.mult), reads=[B_st], writes=[B_st])
                    S.op("act", lambda e: e.activation(out=z, in_=acc[:, cl, :], func=AF.Identity, bias=st[:, 15:16], scale=st[:, 14:15]),
                         reads=[B_st, B_acc[cl]], writes=[B_z])
                    eng2 = "dve" if cl % 2 == 0 else "pool"
                    S.op(eng2, lambda e: e.tensor_tensor(out=z, in0=z, in1=bce[:, 0, :], op=ALU.mult), reads=[B_bce], writes=[B_z])
                    S.op(eng2, lambda e: e.tensor_tensor(out=z, in0=z, in1=bce[:, 1, :], op=ALU.add), reads=[B_bce], writes=[B_z])
                    S.dma("sp", out_d[c * 128:(c + 1) * 128, :], z, reads=[B_z], writes=[B_out[c]])

                def phaseB(e_, t, B_a, aT):
                    B1, w1, B3, w3, B2, w2 = wts[e_]
                    for sub in range(NSUB):
                        cl = t * NSUB + sub
                        i = phi["o"]; phi["o"] = (i + 1) % 2
                        B_po, po = B_PO[i], PO[i][:]

                        def fn(e, sub=sub, po=po):
                            for hf in range(2):
                                for j in range(4):
                                    ins = e.matmul(po[:, hf * 512:(hf + 1) * 512], lhsT=aT[:, j, sub * 128:(sub + 1) * 128], rhs=w2[:, j, hf * 512:(hf + 1) * 512],
                                                   start=(j == 0), stop=(j == 3))
                            return ins
                        S.op("pe", fn, reads=[B_a, B2], writes=[B_po])
                        S.op("dve", lambda e, cl=cl, po=po: e.scalar_tensor_tensor(out=acc[:, cl, :], in0=po, scalar=comb[:, cl, e_:e_ + 1], in1=acc[:, cl, :],
                                                                                   op0=ALU.mult, op1=ALU.add),
                             reads=[B_po, B_comb[cl]], writes=[B_acc[cl]])

                if st_i == 0:
                    load_w(0)
                prev = None
                for ui, (e_, t) in enumerate(units):
                    B_a, aT = phaseA(e_, t)
                    if prev is not None:
                        phaseB(*prev)
                    prev = (e_, t, B_a, aT)
                    if t == 0 and e_ + 1 < NE:
                        load_w(e_ + 1)
                    elif t == 0 and e_ + 1 == NE and st_i + 1 < NST:
                        load_w(0)
                phaseB(*prev)
                if st_i + 1 < NST:
                    load_h2(c0 + TSC)
                run_pipeline([(lambda cl=cl: final_chunk(cl)) for cl in range(TSC)], 3, 4)
            S.wait_all_dma("sp")
            S.flush()


def _consts():
    s = np.arange(128)
    ident = np.eye(128, dtype=np.float32)
    v = np.float32(-1.0 / 16.0)
    texf = np.where(s[:, None] > s[None, :], v, np.float32(0)).astype(np.float32)
    texb = np.where(s[:, None] < s[None, :], v, np.float32(0)).astype(np.float32)
    tincf = np.where(s[:, None] <= s[None, :], v, np.float32(0)).astype(np.float32)
    tincb = np.where(s[:, None] >= s[None, :], v, np.float32(0)).astype(np.float32)
    m = np.where(s[:, None] < s[None, :], 1.0, 0.0) + 2.0 * np.eye(128)
    mfp = np.tile(m.astype(np.float32), (1, 4))
    mbs = np.tile((s[:, None] > s[None, :]).astype(np.int32), (1, 4))
    negcol = np.full((128, 2), v, dtype=np.float32)
    return dict(ident=ident, texf=texf, texb=texb, tincf=tincf, tincb=tincb, mfp=mfp, mbs=mbs, negcol=negcol)


def _colk(v):
    return np.ascontiguousarray(np.asarray(v, dtype=np.float32).reshape(8, 128).T)


def make_in_maps(inp, nb, SEQ):
    f = lambda a: np.ascontiguousarray(np.asarray(a, dtype=np.float32))
    b_ada = f(inp["b_ada"])[0]
    bada4 = np.concatenate([_colk(b_ada[i * D:(i + 1) * D]) for i in (0, 1, 3, 4)], axis=1)
    lncols = np.stack([_colk(inp["ln_in_g"]), _colk(inp["ln_in_b"]), _colk(f(inp["ln1_g"])[0]), _colk(f(inp["ln1_b"])[0])], axis=2)
    lnrows = np.stack([f(inp["ln_in_g"]), f(inp["ln_in_b"]), f(inp["ln1_g"])[0], f(inp["ln1_b"])[0], f(inp["ln2_g"])[0], f(inp["ln2_b"])[0]], axis=0)
    cw = f(inp["conv_w"])[0]
    cb = f(inp["conv_b"])[0]
    convcols = np.zeros((128, 4, 4), np.float32)
    for t in range(3):
        convcols[:, :, t] = cw[t].reshape(4, 128).T
    convcols[:, :, 3] = cb.reshape(4, 128).T
    gw_aug = np.zeros((33, 512), np.float32)
    gw_aug[0:16, 0:256] = f(inp["gate_w2_fwd"])[0]
    gw_aug[16:32, 256:512] = f(inp["gate_w2_bwd"])[0]
    gw_aug[32, 0:256] = f(inp["gate_b_fwd"])[0]
    gw_aug[32, 256:512] = f(inp["gate_b_bwd"])[0]
    wr = np.concatenate([f(inp["router_group_w"])[0], f(inp["router_expert_w"])[0]], axis=1)
    br = np.concatenate([f(inp["router_group_b"])[0], f(inp["router_expert_b"])[0]], axis=0)[None, :]
    shared = dict(
        w_ada=f(inp["w_ada"])[0], bada4=np.ascontiguousarray(bada4), bada_row=np.ascontiguousarray(b_ada[None, :]),
        lncols=np.ascontiguousarray(lncols), lnrows=np.ascontiguousarray(lnrows), w_in=f(inp["w_in"])[0],
        convcols=convcols, gw_aug=gw_aug, normg_col=np.ascontiguousarray(f(inp["gla_norm_g"])[0][:, None]),
        w_out=f(inp["w_out"])[0], wr=np.ascontiguousarray(wr), br_row=np.ascontiguousarray(br),
        ew1=f(inp["expert_w1"])[0], ew3=f(inp["expert_w3"])[0], ew2=f(inp["expert_w2"])[0],
    )
    shared.update(_consts())
    x = f(inp["x"]); c = f(inp["c"]); ctx = f(inp["ctx"]); c_ctx = f(inp["c_ctx"])
    maps = []
    for b in range(nb):
        m = dict(shared)
        m["x"] = np.ascontiguousarray(x[b, :SEQ])
        m["ctx"] = np.ascontiguousarray(ctx[b])
        m["ccol"] = np.ascontiguousarray(np.stack([_colk(c[b]), _colk(c_ctx)], axis=2))
        maps.append(m)
    return maps


_CACHE = {}


def kernel(**inputs):
    x = np.asarray(inputs["x"])
    nb, SEQ = x.shape[0], x.shape[1]
    key = (SEQ,)
    if key not in _CACHE:
        _CACHE[key] = build_program(SEQ, 8 if SEQ >= 1024 else SEQ // 128)
    nc = _CACHE[key]
    maps = make_in_maps(inputs, nb, SEQ)
    res = run_bass_kernel_spmd(nc, maps, core_ids=list(range(nb)))
    out = np.stack([np.asarray(r["out"], dtype=np.float32) for r in res.results], axis=0)
    return out
```
